# Optimizing a Trainium2 kernel written in Bass

```python
import jax
import jax.numpy as jnp
from jax import lax
import numpy as np

D_MODEL = 1024
BATCH = 32
SEQ = 2048
DEPTH = 4

CHUNK = 64
N_MEM = 256
ROPE_THETA = 10000.0
NORM_EPS = 1e-6
NEG_INF = -1e30

RWKV_HEADS = 8
RWKV_HEAD_DIM = 64
RWKV_DIM = RWKV_HEADS * RWKV_HEAD_DIM
RWKV_DECAY_RANK = 64
RWKV_A_RANK = 64
RWKV_V_RANK = 32
RWKV_GATE_RANK = 128
RWKV_LNX_EPS = 1e-5 * RWKV_HEAD_DIM
RWKV_SIZES = (RWKV_DIM, RWKV_DIM, RWKV_DIM, RWKV_DECAY_RANK, RWKV_A_RANK, RWKV_GATE_RANK)
RWKV_COLS = sum(RWKV_SIZES)

DSA_HEADS = 8
DSA_HEAD_DIM = 64
DSA_DIM = DSA_HEADS * DSA_HEAD_DIM
IDX_HEADS = 8
IDX_DIM = 64
DSA_TOPK_MAX = 256
Q_BLOCK = 128
DSA_SIZES = (DSA_DIM, DSA_HEAD_DIM, DSA_HEAD_DIM, IDX_HEADS * IDX_DIM, IDX_DIM, IDX_HEADS)
DSA_COLS = sum(DSA_SIZES)

IN_SIZES = (RWKV_COLS, DSA_COLS, D_MODEL, D_MODEL)
N_IN = sum(IN_SIZES)

MEM_HEADS = 4
MEM_HEAD_DIM = 128
MEM_DIM = MEM_HEADS * MEM_HEAD_DIM

FFN_DIM = 2816
N_EXPERTS = 8
TOP_K = 2
EXPERT_DIM = 1408
N_DENSE = (DEPTH + 1) // 2
N_MOE = DEPTH // 2

kernel_name = 'hybrid_rwkv7_dsa_memxattn_moe_trunk'


def split_cols(t, sizes):
    return jnp.split(t, np.cumsum(sizes)[:-1].tolist(), axis=-1)


def rms_norm(x, g):
    x32 = x.astype(jnp.float32)
    y = x32 * lax.rsqrt(jnp.mean(x32 * x32, axis=-1, keepdims=True) + NORM_EPS)
    return (y * g.astype(jnp.float32)).astype(x.dtype)


def rope_tables(positions, dim):
    inv_freq = 1.0 / (ROPE_THETA ** (jnp.arange(0, dim, 2, dtype=jnp.float32) / dim))
    ang = positions.astype(jnp.float32)[..., None] * inv_freq
    return jnp.cos(ang), jnp.sin(ang)


def apply_rope(x, cos, sin):
    shape = cos.shape[:2] + (1,) * (x.ndim - 3) + cos.shape[2:]
    c, s = cos.reshape(shape), sin.reshape(shape)
    x1, x2 = jnp.split(x.astype(jnp.float32), 2, axis=-1)
    return jnp.concatenate([x1 * c - x2 * s, x2 * c + x1 * s], axis=-1).astype(x.dtype)


def token_shift(t):
    return jnp.pad(t[:, :-1], ((0, 0), (1, 0), (0, 0)))


def rwkv7_scan(r, w, k, v, a, b):
    bsz, _, h, n = r.shape
    xs = tuple(jnp.moveaxis(t, 1, 0) for t in (r, w, k, v, a, b))

    def step(state, inp):
        r_t, w_t, k_t, v_t, a_t, b_t = inp
        sa = jnp.einsum('bhvk,bhk->bhv', state, a_t)
        state = (state * w_t[:, :, None, :] + sa[..., None] * b_t[:, :, None, :]
                 + v_t[..., None] * k_t[:, :, None, :])
        return state, jnp.einsum('bhvk,bhk->bhv', state, r_t)

    _, ys = lax.scan(step, jnp.zeros((bsz, h, n, n), jnp.float32), xs)
    return jnp.moveaxis(ys, 0, 1)


def rwkv7_branch(cols, v_first, mu, w0, w2, a0, a2, g2, v_mix, k_k, k_a, r_k, lnx_g, lnx_b):
    bsz, seqlen, _ = cols.shape
    cols = cols + (token_shift(cols) - cols) * mu
    r, k, v, wd, ad, gd = split_cols(cols, RWKV_SIZES)
    w_log = -jax.nn.softplus(-(w0 + jnp.tanh(wd) @ w2)) - 0.5
    a = jax.nn.sigmoid(a0 + ad @ a2)
    g = jax.nn.sigmoid(gd) @ g2
    if v_mix is None:
        v_first = v
    else:
        v0, v1, v2 = v_mix
        v = v + (v_first - v) * jax.nn.sigmoid(v0 + (v @ v1) @ v2)
    heads = lambda t: t.astype(jnp.float32).reshape(bsz, seqlen, RWKV_HEADS, RWKV_HEAD_DIM)
    kk = heads(k * k_k)
    kk = kk / jnp.maximum(jnp.sqrt(jnp.sum(kk * kk, axis=-1, keepdims=True)), 1e-12)
    k = k * (1.0 + (a - 1.0) * k_a)
    rh, kh, vh, ah = heads(r), heads(k), heads(v), heads(a)
    decay = jnp.exp(-jnp.exp(heads(w_log)))
    y = rwkv7_scan(rh, decay, kh, vh, -kk, kk * ah)
    mean = jnp.mean(y, axis=-1, keepdims=True)
    var = jnp.mean(jnp.square(y - mean), axis=-1, keepdims=True)
    y = (y - mean) * lax.rsqrt(var + RWKV_LNX_EPS)
    y = y.reshape(bsz, seqlen, RWKV_DIM) * lnx_g.astype(jnp.float32) + lnx_b.astype(jnp.float32)
    bonus = jnp.sum(rh * kh * r_k.astype(jnp.float32), axis=-1, keepdims=True) * vh
    y = y + bonus.reshape(bsz, seqlen, RWKV_DIM)
    return (y * g.astype(jnp.float32)).astype(cols.dtype), v_first


def dsa_attention(q, k, v, q_idx, k_idx, w_idx):
    length = q.shape[1]
    top_k = min(DSA_TOPK_MAX, length // 4)
    key_pos = jnp.arange(length)
    gather = jax.vmap(lambda t, i: t[i])
    k_idx32 = k_idx.astype(jnp.float32)

    def query_block(i):
        start = i * Q_BLOCK
        qb = lax.dynamic_slice_in_dim(q, start, Q_BLOCK, axis=1)
        qib = lax.dynamic_slice_in_dim(q_idx, start, Q_BLOCK, axis=1).astype(jnp.float32)
        wb = lax.dynamic_slice_in_dim(w_idx, start, Q_BLOCK, axis=1).astype(jnp.float32)
        limit = ((start + jnp.arange(Q_BLOCK)) // CHUNK + 1) * CHUNK
        admissible = key_pos[None, :] < limit[:, None]
        logits = jnp.einsum('bqhd,bsd->bqhs', qib, k_idx32) * (IDX_DIM ** -0.5)
        score = jnp.einsum('bqhs,bqh->bqs', jax.nn.relu(logits), wb) * (IDX_HEADS ** -0.5)
        score = jnp.where(admissible[None], score, NEG_INF)
        _, sel = lax.top_k(score, top_k)
        valid = sel < limit[None, :, None]
        ks, vs = gather(k, sel), gather(v, sel)
        s = jnp.einsum('bqhd,bqkd->bqhk', qb, ks).astype(jnp.float32) * (DSA_HEAD_DIM ** -0.5)
        p = jax.nn.softmax(jnp.where(valid[:, :, None, :], s, NEG_INF), axis=-1)
        return jnp.einsum('bqhk,bqkd->bqhd', p.astype(vs.dtype), vs)

    out = lax.map(query_block, jnp.arange(length // Q_BLOCK))
    return jnp.moveaxis(out, 0, 1).reshape(q.shape)


def dsa_branch(cols, cos_q, sin_q, cos_i, sin_i, q_norm, k_norm, idx_k_norm):
    bsz, seqlen, _ = cols.shape
    q, k, v, iq, ik, iw = split_cols(cols, DSA_SIZES)
    q = apply_rope(rms_norm(q.reshape(bsz, seqlen, DSA_HEADS, DSA_HEAD_DIM), q_norm), cos_q, sin_q)
    k = apply_rope(rms_norm(k, k_norm), cos_q, sin_q)
    iq = apply_rope(iq.reshape(bsz, seqlen, IDX_HEADS, IDX_DIM), cos_i, sin_i)
    ik = apply_rope(rms_norm(ik, idx_k_norm), cos_i, sin_i)
    return dsa_attention(q, k, v, iq, ik, iw).reshape(bsz, seqlen, DSA_DIM)


def memory_attention(h, mem_n, wq, wkv, q_norm, k_norm, wo):
    bsz, seqlen, _ = h.shape
    n_mem = mem_n.shape[1]
    q = rms_norm((h @ wq).reshape(bsz, seqlen, MEM_HEADS, MEM_HEAD_DIM), q_norm)
    k, v = split_cols(mem_n @ wkv, (MEM_DIM, MEM_DIM))
    k = rms_norm(k.reshape(bsz, n_mem, MEM_HEADS, MEM_HEAD_DIM), k_norm)
    v = v.reshape(bsz, n_mem, MEM_HEADS, MEM_HEAD_DIM)
    s = jnp.einsum('bshd,bmhd->bhsm', q, k).astype(jnp.float32) * (MEM_HEAD_DIM ** -0.5)
    p = jax.nn.softmax(s, axis=-1).astype(v.dtype)
    o = jnp.einsum('bhsm,bmhd->bshd', p, v).reshape(bsz, seqlen, MEM_DIM)
    return o @ wo


def swiglu(h, wg, wu, wd):
    return (jax.nn.silu(h @ wg) * (h @ wu)) @ wd


def moe_swiglu(h, router, bias, wg, wu, wd):
    logits = (h @ router).astype(jnp.float32) + bias.astype(jnp.float32)
    top_logit, top_idx = lax.top_k(logits, TOP_K)
    top_w = jax.nn.softmax(top_logit, axis=-1)
    gates = jnp.sum(jax.nn.one_hot(top_idx, N_EXPERTS, dtype=jnp.float32) * top_w[..., None], axis=-2)
    gates = gates.astype(h.dtype)
    out = jnp.zeros_like(h)
    for e in range(N_EXPERTS):
        out = out + gates[..., e:e + 1] * swiglu(h, wg[e], wu[e], wd[e])
    return out


def setup_inputs(seed: int = 0) -> dict:
    keys = iter(jax.random.split(jax.random.key(seed), 48))
    f32 = jnp.float32

    def normal(shape, scale):
        return jax.random.normal(next(keys), shape, f32) * scale

    def gain(shape):
        return 1.0 + normal(shape, 0.02)

    x = normal((BATCH, SEQ, D_MODEL), 1.0)
    mem = normal((BATCH, N_MEM, D_MODEL), 1.0)
    offsets = jax.random.randint(next(keys), (BATCH, 1), 0, 64) * CHUNK
    positions = (offsets + jnp.arange(SEQ)[None, :]).astype(jnp.int32)
    nv = DEPTH - 1
    return {
        'x': x,
        'mem': mem,
        'positions': positions,
        'norm_mix': gain((DEPTH, D_MODEL)),
        'w_in': normal((DEPTH, D_MODEL, N_IN), D_MODEL ** -0.5),
        'rwkv_mu': jax.random.uniform(next(keys), (DEPTH, RWKV_COLS), f32, 0.0, 1.0),
        'rwkv_w0': jax.random.uniform(next(keys), (DEPTH, RWKV_DIM), f32, -6.0, -1.0),
        'rwkv_w2': normal((DEPTH, RWKV_DECAY_RANK, RWKV_DIM), 0.5 * RWKV_DECAY_RANK ** -0.5),
        'rwkv_a0': normal((DEPTH, RWKV_DIM), 0.1),
        'rwkv_a2': normal((DEPTH, RWKV_A_RANK, RWKV_DIM), RWKV_A_RANK ** -0.5),
        'rwkv_g2': normal((DEPTH, RWKV_GATE_RANK, RWKV_DIM), RWKV_GATE_RANK ** -0.5),
        'rwkv_v0': normal((nv, RWKV_DIM), 0.5),
        'rwkv_v1': normal((nv, RWKV_DIM, RWKV_V_RANK), RWKV_DIM ** -0.5),
        'rwkv_v2': normal((nv, RWKV_V_RANK, RWKV_DIM), RWKV_V_RANK ** -0.5),
        'rwkv_kk': 0.85 + normal((DEPTH, RWKV_DIM), 0.02),
        'rwkv_ka': gain((DEPTH, RWKV_DIM)),
        'rwkv_rk': normal((DEPTH, RWKV_HEADS, RWKV_HEAD_DIM), 0.1),
        'rwkv_lnx_g': gain((DEPTH, RWKV_DIM)),
        'rwkv_lnx_b': normal((DEPTH, RWKV_DIM), 0.02),
        'dsa_q_norm': gain((DEPTH, DSA_HEAD_DIM)),
        'dsa_k_norm': gain((DEPTH, DSA_HEAD_DIM)),
        'idx_k_norm': gain((DEPTH, IDX_DIM)),
        'w_branch_a': normal((DEPTH, RWKV_DIM, D_MODEL), RWKV_DIM ** -0.5),
        'w_branch_b': normal((DEPTH, DSA_DIM, D_MODEL), DSA_DIM ** -0.5),
        'w_out': normal((DEPTH, D_MODEL, D_MODEL), 0.5 * D_MODEL ** -0.5),
        'norm_mem': gain((DEPTH, D_MODEL)),
        'mem_tok_norm': gain((DEPTH, D_MODEL)),
        'mem_wq': normal((DEPTH, D_MODEL, MEM_DIM), D_MODEL ** -0.5),
        'mem_wkv': normal((DEPTH, D_MODEL, 2 * MEM_DIM), D_MODEL ** -0.5),
        'mem_q_norm': gain((DEPTH, MEM_HEAD_DIM)),
        'mem_k_norm': gain((DEPTH, MEM_HEAD_DIM)),
        'mem_wo': normal((DEPTH, MEM_DIM, D_MODEL), 0.5 * MEM_DIM ** -0.5),
        'norm_ffn': gain((DEPTH, D_MODEL)),
        'ffn_wg': normal((N_DENSE, D_MODEL, FFN_DIM), D_MODEL ** -0.5),
        'ffn_wu': normal((N_DENSE, D_MODEL, FFN_DIM), D_MODEL ** -0.5),
        'ffn_wd': normal((N_DENSE, FFN_DIM, D_MODEL), 0.5 * FFN_DIM ** -0.5),
        'moe_router': normal((N_MOE, D_MODEL, N_EXPERTS), D_MODEL ** -0.5),
        'moe_bias': normal((N_MOE, N_EXPERTS), 0.01),
        'moe_wg': normal((N_MOE, N_EXPERTS, D_MODEL, EXPERT_DIM), D_MODEL ** -0.5),
        'moe_wu': normal((N_MOE, N_EXPERTS, D_MODEL, EXPERT_DIM), D_MODEL ** -0.5),
        'moe_wd': normal((N_MOE, N_EXPERTS, EXPERT_DIM, D_MODEL), 0.5 * EXPERT_DIM ** -0.5),
    }


def reference(x, mem, positions, norm_mix, w_in, rwkv_mu, rwkv_w0, rwkv_w2, rwkv_a0, rwkv_a2,
              rwkv_g2, rwkv_v0, rwkv_v1, rwkv_v2, rwkv_kk, rwkv_ka, rwkv_rk, rwkv_lnx_g, rwkv_lnx_b,
              dsa_q_norm, dsa_k_norm, idx_k_norm, w_branch_a, w_branch_b, w_out, norm_mem,
              mem_tok_norm, mem_wq, mem_wkv, mem_q_norm, mem_k_norm, mem_wo, norm_ffn, ffn_wg,
              ffn_wu, ffn_wd, moe_router, moe_bias, moe_wg, moe_wu, moe_wd):
    cos_q, sin_q = rope_tables(positions, DSA_HEAD_DIM)
    cos_i, sin_i = rope_tables(positions, IDX_DIM)
    v_first = None
    for l in range(DEPTH):
        h = rms_norm(x, norm_mix[l])
        cols_a, cols_b, gate_a, gate_b = split_cols(h @ w_in[l], IN_SIZES)
        v_mix = None if l == 0 else (rwkv_v0[l - 1], rwkv_v1[l - 1], rwkv_v2[l - 1])
        y_a, v_first = rwkv7_branch(cols_a, v_first, rwkv_mu[l], rwkv_w0[l], rwkv_w2[l], rwkv_a0[l],
                                    rwkv_a2[l], rwkv_g2[l], v_mix, rwkv_kk[l], rwkv_ka[l], rwkv_rk[l],
                                    rwkv_lnx_g[l], rwkv_lnx_b[l])
        y_b = dsa_branch(cols_b, cos_q, sin_q, cos_i, sin_i, dsa_q_norm[l], dsa_k_norm[l], idx_k_norm[l])
        merged = (jax.nn.sigmoid(gate_a) * (y_a @ w_branch_a[l])
                  + jax.nn.sigmoid(gate_b) * (y_b @ w_branch_b[l]))
        x = x + merged @ w_out[l]
        x = x + memory_attention(rms_norm(x, norm_mem[l]), rms_norm(mem, mem_tok_norm[l]), mem_wq[l],
                                 mem_wkv[l], mem_q_norm[l], mem_k_norm[l], mem_wo[l])
        h = rms_norm(x, norm_ffn[l])
        j = l // 2
        if l % 2 == 0:
            x = x + swiglu(h, ffn_wg[j], ffn_wu[j], ffn_wd[j])
        else:
            x = x + moe_swiglu(h, moe_router[j], moe_bias[j], moe_wg[j], moe_wu[j], moe_wd[j])
    return x
```

```python
import numpy as np
from contextlib import ExitStack
import concourse.bass as bass
import concourse.mybir as mybir
from concourse.bass_utils import run_bass_kernel_spmd

F32 = mybir.dt.float32
BF16 = mybir.dt.bfloat16
I32 = mybir.dt.int32
ALU = mybir.AluOpType
AF = mybir.ActivationFunctionType
AX = mybir.AxisListType

SAME_ENGINE_SYNC = True


class Tok:
    __slots__ = ("w", "r")

    def __init__(self):
        self.w = None
        self.r = {}


class Buf:
    __slots__ = ("ap", "t")

    def __init__(self, ap, t=None):
        self.ap = ap
        self.t = t if t is not None else Tok()

    def __getitem__(self, k):
        return self.ap[k]


class KB:
    def __init__(self, nc, arena_f32=48000):
        self.nc = nc
        self.st = ExitStack()
        self.eng = {"pe": nc.tensor, "act": nc.scalar, "dve": nc.vector, "pool": nc.gpsimd, "sp": nc.sync}
        self.prog = {e: [] for e in self.eng}
        self.sems = {}
        self.cnt = {}
        self.seen = {e: {} for e in self.eng}
        self.arena = self.st.enter_context(nc.sbuf_tensor("arena", [128, arena_f32], F32))
        self.top = 0
        self.arena_n = arena_f32
        self.banks = [self.st.enter_context(nc.psum_tensor(f"psb{i}", [128, 512], F32)) for i in range(8)]
        self.bank_tok = [Tok() for _ in range(8)]
        self.bank_i = 0
        self.reserved = set()
        self.dma_rr = {}
        self.nops = 0

    def alloc(self, n, dtype=F32):
        nf = n if dtype in (F32, I32) else (n + 1) // 2
        off = self.top
        self.top += nf
        assert self.top <= self.arena_n, f"arena overflow {self.top}"
        ap = self.arena[:, off:off + nf]
        if dtype != F32:
            ap = ap.bitcast(dtype)
            if ap.shape[-1] != n:
                ap = ap[:, 0:n]
        return Buf(ap)

    def mark(self):
        return self.top

    def release(self, m):
        self.barrier()
        self.top = m

    def psum(self):
        while True:
            i = self.bank_i
            self.bank_i = (i + 1) % 8
            if i not in self.reserved:
                return Buf(self.banks[i][:], self.bank_tok[i])

    def psum_fixed(self, i):
        return Buf(self.banks[i][:], self.bank_tok[i])

    def _sem(self, key):
        if key not in self.sems:
            name = key if isinstance(key, str) else key[0] + "_" + key[1]
            self.sems[key] = self.st.enter_context(self.nc.semaphore("s_" + name))
            self.cnt[key] = 0
        return self.sems[key]

    def _wait(self, eng, deps):
        for key, val in deps:
            if key == eng and (eng == "pe" or not SAME_ENGINE_SYNC):
                continue
            if isinstance(key, tuple):
                val = max(val, self.cnt[key])
            if self.seen[eng].get(key, 0) >= val:
                continue
            self.seen[eng][key] = val
            self.prog[eng].append(("w", key, val))

    def _deps(self, reads, writes):
        deps = []
        for t in reads:
            if t.w:
                deps.append(t.w)
        for t in writes:
            if t.w:
                deps.append(t.w)
            deps.extend(t.r.items())
        return deps

    @staticmethod
    def _toks(xs):
        return [x.t if isinstance(x, Buf) else x for x in xs]

    def op(self, eng, fn, reads=(), writes=()):
        reads = self._toks(reads)
        writes = self._toks(writes)
        self._wait(eng, self._deps(reads, writes))
        self._sem(eng)
        self.cnt[eng] += 1
        v = self.cnt[eng]
        self.prog[eng].append(("o", fn, eng, 1))
        for t in reads:
            t.r[eng] = v
        for t in writes:
            t.w = (eng, v)
            t.r = {}
        self.nops += 1

    NDMASEM = 8

    def dma(self, eng, out, in_, reads=(), writes=(), ch="a", slow=False):
        reads = self._toks(reads)
        writes = self._toks(writes)
        i = self.dma_rr.get(eng, 0)
        self.dma_rr[eng] = (i + 1) % self.NDMASEM
        key = (eng, str(i))
        self._sem(key)
        self._wait(eng, self._deps(reads, writes) + [(key, self.cnt[key])])
        self.cnt[key] += 16
        v = self.cnt[key]
        kw = {"allow_slow_non_contiguous": True} if slow else {}
        self.prog[eng].append(("o", lambda e, out=out, in_=in_: e.dma_start(out=out, in_=in_, **kw), key, 16))
        for t in reads:
            t.r[key] = v
        for t in writes:
            t.w = (key, v)
            t.r = {}
        self.nops += 1

    def barrier(self):
        allk = [(k, c) for k, c in self.cnt.items() if c > 0]
        for e in self.eng:
            self._wait(e, [(k, c) for k, c in allk if k != e])

    def finish(self):
        self.barrier()
        with self.nc.Block() as block:
            for name, deco in (("sp", block.sync), ("act", block.scalar), ("dve", block.vector),
                               ("pool", block.gpsimd), ("pe", block.tensor)):
                def f(e, name=name):
                    for it in self.prog[name]:
                        if it[0] == "w":
                            e.wait_ge(self.sems[it[1]], it[2])
                        else:
                            it[1](e).then_inc(self.sems[it[2]], it[3])
                deco(f)
        self.st.close()

    def mm(self, out, lhsT, rhs, start=True, stop=True, reads=(), writes=()):
        self.op("pe", lambda e: e.matmul(out, lhsT=lhsT, rhs=rhs, start=start, stop=stop),
                reads=reads, writes=writes)

    def act(self, out, in_, func, reads=(), writes=(), scale=None, bias=None, accum=None):
        kw = {}
        if scale is not None:
            kw["scale"] = scale
        if bias is not None:
            kw["bias"] = bias
        if accum is not None:
            kw["accum_out"] = accum
        self.op("act", lambda e: e.activation(out=out, in_=in_, func=func, **kw), reads=reads, writes=writes)

    def tt(self, eng, out, in0, in1, op, reads=(), writes=()):
        self.op(eng, lambda e: e.tensor_tensor(out=out, in0=in0, in1=in1, op=op), reads=reads, writes=writes)

    def ts(self, eng, out, in0, s1, s2, op0, op1=None, reads=(), writes=(), accum=None):
        kw = {}
        if op1 is not None:
            kw["op1"] = op1
        if accum is not None:
            kw["accum_out"] = accum
        self.op(eng, lambda e: e.tensor_scalar(out=out, in0=in0, scalar1=s1, scalar2=s2, op0=op0, **kw),
                reads=reads, writes=writes)

    def stt(self, eng, out, in0, scalar, in1, op0, op1, reads=(), writes=()):
        self.op(eng, lambda e: e.scalar_tensor_tensor(out=out, in0=in0, scalar=scalar, in1=in1, op0=op0, op1=op1),
                reads=reads, writes=writes)

    def copy(self, eng, out, in_, reads=(), writes=()):
        if eng == "act":
            self.op(eng, lambda e: e.activation(out=out, in_=in_, func=AF.Copy), reads=reads, writes=writes)
        else:
            self.op(eng, lambda e: e.tensor_copy(out=out, in_=in_), reads=reads, writes=writes)

    def memset(self, eng, out, val, writes=()):
        self.op(eng, lambda e: e.memset(out, val), writes=writes)

    def aselect(self, out, in_, pattern, cmp, fill, base, cm, reads=(), writes=()):
        self.op("pool", lambda e: e.affine_select(out=out, in_=in_, pattern=pattern, compare_op=cmp, fill=fill,
                                                  base=base, channel_multiplier=cm), reads=reads, writes=writes)

T = 2048
D = 1024
TG = 512
NTG = 4
EPS = 1e-6

WSPEC = [
    ("norm_mix", (4, 1024)), ("w_in", (4, 1024, 5064)), ("rwkv_mu", (4, 1792)), ("rwkv_w0", (4, 512)),
    ("rwkv_w2", (4, 64, 512)), ("rwkv_a0", (4, 512)), ("rwkv_a2", (4, 64, 512)), ("rwkv_g2", (4, 128, 512)),
    ("rwkv_v0", (3, 512)), ("rwkv_v1", (3, 512, 32)), ("rwkv_v2", (3, 32, 512)), ("rwkv_kk", (4, 512)),
    ("rwkv_ka", (4, 512)), ("rwkv_rk", (4, 8, 64)), ("rwkv_lnx_g", (4, 512)), ("rwkv_lnx_b", (4, 512)),
    ("dsa_q_norm", (4, 64)), ("dsa_k_norm", (4, 64)), ("idx_k_norm", (4, 64)),
    ("w_branch_a", (4, 512, 1024)), ("w_branch_b", (4, 512, 1024)), ("w_out", (4, 1024, 1024)),
    ("norm_mem", (4, 1024)), ("mem_tok_norm", (4, 1024)), ("mem_wq", (4, 1024, 512)), ("mem_wkv", (4, 1024, 1024)),
    ("mem_q_norm", (4, 128)), ("mem_k_norm", (4, 128)), ("mem_wo", (4, 512, 1024)), ("norm_ffn", (4, 1024)),
    ("ffn_wg", (2, 1024, 2816)), ("ffn_wu", (2, 1024, 2816)), ("ffn_wd", (2, 2816, 1024)),
    ("moe_router", (2, 1024, 8)), ("moe_bias", (2, 8)), ("moe_wg", (2, 8, 1024, 1408)),
    ("moe_wu", (2, 8, 1024, 1408)), ("moe_wd", (2, 8, 1408, 1024)),
]


class G:
    def __getattr__(self, name):
        shp = dict(WSPEC).get(name)
        if shp is None:
            raise AttributeError(name)
        ap = self.nc.dram_tensor(name, list(shp), F32, kind="ExternalInput").ap()
        self.W[name] = ap
        setattr(self, name, ap)
        return ap


def v3(ap, a):
    return ap.rearrange("p (a b) -> p a b", a=a)


def setup(nc, NS, dbg, ext_in=()):
    g = G()
    g.NS = NS
    g.x = nc.dram_tensor("x", [NS, T, D], F32, kind="ExternalInput").ap()
    g.mem = nc.dram_tensor("mem", [NS, 256, D], F32, kind="ExternalInput").ap()
    g.pos = nc.dram_tensor("pos", [NS, T], I32, kind="ExternalInput").ap()
    g.nc = nc
    g.W = {}
    g.out = nc.dram_tensor("out", [NS, T, D], F32, kind="ExternalOutput").ap()
    def kd(n):
        return "ExternalInput" if n in ext_in else ("ExternalOutput" if dbg else "Internal")
    g.xT = nc.dram_tensor("xT", [D, T], F32, kind=kd("xT")).ap()
    g.projT = nc.dram_tensor("projT", [5064, T], BF16, kind=kd("projT")).ap()
    g.vfT = nc.dram_tensor("vfT", [512, T], BF16, kind=kd("vfT")).ap()
    g.yaT = nc.dram_tensor("yaT", [512, T], BF16, kind=kd("yaT")).ap()
    g.ybT = nc.dram_tensor("ybT", [512, T], BF16, kind=kd("ybT")).ap()
    g.ropeT = nc.dram_tensor("ropeT", [2, 64, T], F32, kind=kd("ropeT")).ap()
    g.dbg = nc.dram_tensor("dbg", [128, 8192], F32, kind=kd("dbg")).ap()
    g.xtok = [[Tok() for _ in range(NTG)] for _ in range(8)]
    g.ptok = [Tok() for _ in range(40)]
    g.vftok = Tok()
    g.yatok = Tok()
    g.ybtok = Tok()
    g.ropetok = Tok()
    g.dbgtok = Tok()
    return g


def xtoks(g, kcs=range(8), tgs=range(NTG)):
    return [g.xtok[a][b] for a in kcs for b in tgs]


def ptoks(g, r0, r1):
    return [g.ptok[i] for i in range(r0 // 128, (r1 - 1) // 128 + 1)]


def setup_consts(k, g):
    c = G()
    g.c = c
    c.ident_f = k.alloc(128)
    c.ident_b = k.alloc(128, BF16)
    c.ones_f = k.alloc(128)
    c.ones_b = k.alloc(128, BF16)
    k.memset("pool", c.ones_f[:, :], 1.0, writes=[c.ones_f])
    k.memset("pool", c.ones_b[:, :], 1.0, writes=[c.ones_b])
    k.memset("pool", c.ident_f[:, :], 1.0, writes=[c.ident_f])
    k.aselect(c.ident_f[:, :], c.ident_f[:, :], [[-1, 128]], ALU.is_equal, 0.0, 0, 1, reads=[c.ident_f], writes=[c.ident_f])
    k.copy("dve", c.ident_b[:, :], c.ident_f[:, :], reads=[c.ident_f], writes=[c.ident_b])
    c.gains = k.alloc(128)
    for i, nm in enumerate(("norm_mix", "norm_mem", "mem_tok_norm", "norm_ffn")):
        src = getattr(g, nm).rearrange("l (kc p) -> p l kc", p=128)
        k.dma("sp", v3(c.gains[:, i * 32:(i + 1) * 32], 4), src, writes=[c.gains], slow=True)
    return c


def gain_ap(g, kind, l):
    o = kind * 32 + l * 8
    return g.c.gains[:, o:o + 8]


class NormCtx:
    def __init__(self, k, g, own_x=True):
        self.k = k
        self.g = g
        self.x32 = [k.alloc(8 * TG) for _ in range(2)] if own_x else None
        self.sq = k.alloc(8 * TG, BF16)
        self.r1 = k.alloc(TG)
        self.rstd = k.alloc(TG)
        self.i = 0

    def run(self, tg, hT3_out, h_tok, x3=None, xtok=None, h32_out=None):
        k, g = self.k, self.g
        if x3 is None:
            xb = self.x32[self.i % 2]
            x3, xtok = v3(xb[:, :], 8), xb
        self.i += 1
        xTv = g.xT.rearrange("(kc p) t -> p kc t", p=128)
        sl = slice(tg * TG, (tg + 1) * TG)
        k.dma("sp", x3, xTv[:, :, sl], reads=xtoks(g, tgs=[tg]), writes=[xtok])
        sq3 = v3(self.sq[:, :], 8)
        k.act(sq3, x3, AF.Square, reads=[xtok], writes=[self.sq])
        ps = k.psum()
        for kc in range(8):
            k.mm(ps[:, :], lhsT=g.c.ones_b[:, :], rhs=sq3[:, kc, :], start=(kc == 0), stop=(kc == 7),
                 reads=[g.c.ones_b, self.sq], writes=[ps])
        k.act(self.r1[:, :], ps[:, :], AF.Ln, scale=1.0 / D, bias=EPS, reads=[ps], writes=[self.r1])
        k.act(self.rstd[:, :], self.r1[:, :], AF.Exp, scale=-0.5, reads=[self.r1], writes=[self.rstd])
        k.tt("dve", hT3_out, x3, self.rstd[:, :].unsqueeze(1).to_broadcast([128, 8, TG]), ALU.mult,
             reads=[xtok, self.rstd], writes=[h_tok])
        if h32_out is not None:
            k.tt("pool", v3(h32_out[:, :], 8), x3, self.rstd[:, :].unsqueeze(1).to_broadcast([128, 8, TG]),
                 ALU.mult, reads=[xtok, self.rstd], writes=[h32_out])
        return x3, xtok


def stage_x0(k, g, s):
    m = k.mark()
    xin = [k.alloc(1024) for _ in range(2)]
    st = [k.alloc(1024) for _ in range(2)]
    xTv = g.xT.rearrange("(kc p) t -> p kc t", p=128)
    for tt in range(16):
        xb = xin[tt % 2]
        sb = st[tt % 2]
        k.dma("sp", xb[:, :], g.x[s, tt * 128:(tt + 1) * 128, :], writes=[xb])
        for half in range(2):
            ps = k.psum()
            for j in range(4):
                kc = half * 4 + j
                k.mm(ps[:, j * 128:(j + 1) * 128], lhsT=xb[:, kc * 128:(kc + 1) * 128], rhs=g.c.ident_f[:, :],
                     reads=[xb, g.c.ident_f], writes=[ps])
            k.copy("act" if half == 0 else "dve", sb[:, half * 512:(half + 1) * 512], ps[:, :], reads=[ps], writes=[sb])
        k.dma("pool", xTv[:, :, tt * 128:(tt + 1) * 128], v3(sb[:, :], 8), reads=[sb], writes=xtoks(g, tgs=[tt // 4]))
    k.release(m)


def stage_final(k, g, s):
    m = k.mark()
    xin = [k.alloc(1024) for _ in range(2)]
    st = [k.alloc(1024) for _ in range(2)]
    xTv = g.xT.rearrange("(kc p) t -> p kc t", p=128)
    for tt in range(16):
        xb = xin[tt % 2]
        sb = st[tt % 2]
        k.dma("sp", v3(xb[:, :], 8), xTv[:, :, tt * 128:(tt + 1) * 128], reads=xtoks(g, tgs=[tt // 4]), writes=[xb])
        for half in range(2):
            ps = k.psum()
            for j in range(4):
                kc = half * 4 + j
                k.mm(ps[:, j * 128:(j + 1) * 128], lhsT=xb[:, kc * 128:(kc + 1) * 128], rhs=g.c.ident_f[:, :],
                     reads=[xb, g.c.ident_f], writes=[ps])
            k.copy("act" if half == 0 else "dve", sb[:, half * 512:(half + 1) * 512], ps[:, :], reads=[ps], writes=[sb])
        k.dma("pool", g.out[s, tt * 128:(tt + 1) * 128, :], sb[:, :], reads=[sb], writes=[g.outtok])
    k.release(m)


def stage_a(k, g, l):
    m = k.mark()
    hT = k.alloc(8 * T, BF16)
    hT3 = v3(hT[:, :], 8)
    nx = NormCtx(k, g)
    for tg in range(NTG):
        nx.run(tg, hT3[:, :, tg * TG:(tg + 1) * TG], hT)
    wst = [k.alloc(8 * 512) for _ in range(2)]
    wbf = [k.alloc(8 * 512, BF16) for _ in range(2)]
    ost = [k.alloc(T, BF16) for _ in range(2)]
    w_v = g.w_in[l].rearrange("(kc p) c -> p kc c", p=128)
    gn = gain_ap(g, 0, l)
    blk = 0
    for cg in range(10):
        c0 = cg * 512
        n = min(512, 5064 - c0)
        ws, wb = wst[cg % 2], wbf[cg % 2]
        ws3, wb3 = v3(ws[:, :], 8), v3(wb[:, :], 8)
        k.dma("sp", ws3[:, :, 0:n], w_v[:, :, c0:c0 + n], writes=[ws])
        k.tt("pool" if cg % 2 else "dve", wb3[:, :, 0:n], ws3[:, :, 0:n], gn.unsqueeze(2).to_broadcast([128, 8, n]),
             ALU.mult, reads=[ws, g.c.gains], writes=[wb])
        for cb in range((n + 127) // 128):
            mm_ = min(128, n - cb * 128)
            ob = ost[blk % 2]
            blk += 1
            for tg in range(NTG):
                ps = k.psum()
                for kc in range(8):
                    k.mm(ps[0:mm_, :], lhsT=wb3[:, kc, cb * 128:cb * 128 + mm_], rhs=hT3[:, kc, tg * TG:(tg + 1) * TG],
                         start=(kc == 0), stop=(kc == 7), reads=[wb, hT], writes=[ps])
                k.copy("act" if tg % 2 == 0 else "dve", ob[0:mm_, tg * TG:(tg + 1) * TG], ps[0:mm_, :], reads=[ps], writes=[ob])
            r0 = c0 + cb * 128
            k.dma("pool", g.projT[r0:r0 + mm_, :], ob[0:mm_, :], reads=[ob], writes=ptoks(g, r0, r0 + mm_))
    k.release(m)


def stage_merge(k, g, l):
    m = k.mark()
    wba = k.alloc(8 * 1024, BF16)
    wbb = k.alloc(8 * 1024, BF16)
    wo = k.alloc(8 * 1024, BF16)
    m2 = k.mark()
    stg = [k.alloc(8 * 1024) for _ in range(2)]
    k.dma("sp", v3(stg[0][0:64, :], 8), g.w_branch_a[l].rearrange("(h d) c -> d h c", d=64), writes=[stg[0]])
    k.copy("dve", wba[0:64, :], stg[0][0:64, :], reads=[stg[0]], writes=[wba])
    k.dma("sp", v3(stg[1][0:64, :], 8), g.w_branch_b[l].rearrange("(h d) c -> d h c", d=64), writes=[stg[1]])
    k.copy("pool", wbb[0:64, :], stg[1][0:64, :], reads=[stg[1]], writes=[wbb])
    k.dma("sp", v3(stg[0][:, :], 8), g.w_out[l].rearrange("(kc p) c -> p kc c", p=128), writes=[stg[0]])
    k.copy("dve", wo[:, :], stg[0][:, :], reads=[stg[0]], writes=[wo])
    k.release(m2)
    wba3, wbb3, wo3 = v3(wba[:, :], 8), v3(wbb[:, :], 8), v3(wo[:, :], 8)
    ya = [k.alloc(8 * TG, BF16) for _ in range(2)]
    yb = [k.alloc(8 * TG, BF16) for _ in range(2)]
    ga = [k.alloc(8 * TG, BF16) for _ in range(2)]
    gb = [k.alloc(8 * TG, BF16) for _ in range(2)]
    xb = [k.alloc(8 * TG) for _ in range(2)]
    mg = k.alloc(8 * TG, BF16)
    tA = k.alloc(TG)
    tB = k.alloc(TG)
    xTv = g.xT.rearrange("(kc p) t -> p kc t", p=128)
    GA0, GB0 = 3016, 4040
    for tg in range(NTG):
        i = tg % 2
        sl = slice(tg * TG, (tg + 1) * TG)
        ya3, yb3, ga3, gb3, x3, mg3 = (v3(b[:, :], 8) for b in (ya[i], yb[i], ga[i], gb[i], xb[i], mg))
        k.dma("sp", ya3[0:64], g.yaT.rearrange("(h d) t -> d h t", d=64)[:, :, sl], reads=[g.yatok], writes=[ya[i]])
        k.dma("sp", yb3[0:64], g.ybT.rearrange("(h d) t -> d h t", d=64)[:, :, sl], reads=[g.ybtok], writes=[yb[i]])
        k.dma("sp", ga3, g.projT[GA0:GA0 + 1024, :].rearrange("(kc p) t -> p kc t", p=128)[:, :, sl],
              reads=ptoks(g, GA0, GA0 + 1024), writes=[ga[i]])
        k.dma("sp", gb3, g.projT[GB0:GB0 + 1024, :].rearrange("(kc p) t -> p kc t", p=128)[:, :, sl],
              reads=ptoks(g, GB0, GB0 + 1024), writes=[gb[i]])
        k.dma("sp", x3, xTv[:, :, sl], reads=xtoks(g, tgs=[tg]), writes=[xb[i]])
        k.act(ga[i][:, :], ga[i][:, :], AF.Sigmoid, reads=[ga[i]], writes=[ga[i]])
        k.act(gb[i][:, :], gb[i][:, :], AF.Sigmoid, reads=[gb[i]], writes=[gb[i]])
        for oc in range(8):
            pa = k.psum()
            for h in range(8):
                k.mm(pa[:, :], lhsT=wba3[0:64, h, oc * 128:(oc + 1) * 128], rhs=ya3[0:64, h, :], start=(h == 0), stop=(h == 7),
                     reads=[wba, ya[i]], writes=[pa])
            pb = k.psum()
            for h in range(8):
                k.mm(pb[:, :], lhsT=wbb3[0:64, h, oc * 128:(oc + 1) * 128], rhs=yb3[0:64, h, :], start=(h == 0), stop=(h == 7),
                     reads=[wbb, yb[i]], writes=[pb])
            k.tt("dve", tA[:, :], pa[:, :], ga3[:, oc, :], ALU.mult, reads=[pa, ga[i]], writes=[tA])
            k.tt("dve", tB[:, :], pb[:, :], gb3[:, oc, :], ALU.mult, reads=[pb, gb[i]], writes=[tB])
            k.tt("pool", mg3[:, oc, :], tA[:, :], tB[:, :], ALU.add, reads=[tA, tB], writes=[mg])
        for oc in range(8):
            ps = k.psum()
            for kc in range(8):
                k.mm(ps[:, :], lhsT=wo3[:, kc, oc * 128:(oc + 1) * 128], rhs=mg3[:, kc, :], start=(kc == 0), stop=(kc == 7),
                     reads=[wo, mg], writes=[ps])
            k.tt("dve", x3[:, oc, :], x3[:, oc, :], ps[:, :], ALU.add, reads=[ps, xb[i]], writes=[xb[i]])
        k.dma("pool", xTv[:, :, sl], x3, reads=[xb[i]], writes=xtoks(g, tgs=[tg]))
    k.release(m)


def rs_norm_bcast(k, g, src_f, n, parts, dim, tmp_sq, tmp_r):
    k.act(tmp_sq[0:parts, 0:n], src_f[0:parts, 0:n], AF.Square, reads=[src_f], writes=[tmp_sq])
    ps = k.psum()
    k.mm(ps[0:parts, 0:n], lhsT=g.c.ones_b[0:parts, 0:parts], rhs=tmp_sq[0:parts, 0:n], reads=[tmp_sq, g.c.ones_b], writes=[ps])
    k.act(tmp_r[0:parts, 0:n], ps[0:parts, 0:n], AF.Ln, scale=1.0 / dim, bias=EPS, reads=[ps], writes=[tmp_r])
    k.act(tmp_r[0:parts, 0:n], tmp_r[0:parts, 0:n], AF.Exp, scale=-0.5, reads=[tmp_r], writes=[tmp_r])


def stage_mem(k, g, l, s):
    m = k.mark()
    wq = k.alloc(8 * 512, BF16)
    wkv = k.alloc(8 * 1024, BF16)
    wo = k.alloc(4 * 1024, BF16)
    KT = k.alloc(4 * 256, BF16)
    V = k.alloc(2 * 512, BF16)
    qg = k.alloc(1)
    kg = k.alloc(1)
    k.dma("sp", qg[:, 0:1], g.mem_q_norm[l].rearrange("(p o) -> p o", o=1), writes=[qg])
    k.dma("sp", kg[:, 0:1], g.mem_k_norm[l].rearrange("(p o) -> p o", o=1), writes=[kg])
    k.ts("pool", qg[:, 0:1], qg[:, 0:1], float(128 ** -0.5), None, ALU.mult, reads=[qg], writes=[qg])
    m2 = k.mark()
    stg = [k.alloc(8 * 1024) for _ in range(2)]
    s0, s1 = v3(stg[0][:, :], 8), v3(stg[1][:, :], 8)
    wq3, wkv3, wo3 = v3(wq[:, :], 8), v3(wkv[:, :], 8), v3(wo[:, :], 4)
    k.dma("sp", s0[:, :, 0:512], g.mem_wq[l].rearrange("(kc p) c -> p kc c", p=128), writes=[stg[0]])
    k.tt("dve", wq3, s0[:, :, 0:512], gain_ap(g, 1, l).unsqueeze(2).to_broadcast([128, 8, 512]), ALU.mult,
         reads=[stg[0], g.c.gains], writes=[wq])
    k.dma("sp", s1, g.mem_wkv[l].rearrange("(kc p) c -> p kc c", p=128), writes=[stg[1]])
    k.tt("pool", wkv3, s1, gain_ap(g, 2, l).unsqueeze(2).to_broadcast([128, 8, 1024]), ALU.mult,
         reads=[stg[1], g.c.gains], writes=[wkv])
    k.dma("sp", v3(stg[0][:, 0:4096], 4), g.mem_wo[l].rearrange("(hd p) c -> p hd c", p=128), writes=[stg[0]])
    k.copy("dve", wo[:, :], stg[0][:, 0:4096], reads=[stg[0]], writes=[wo])
    mt_in = stg[1]
    memT = k.alloc(8 * 256)
    hm = k.alloc(8 * 256, BF16)
    memT3, hm3 = v3(memT[:, :], 8), v3(hm[:, :], 8)
    for t2 in range(2):
        k.dma("sp", mt_in[:, t2 * 1024:(t2 + 1) * 1024], g.mem[s, t2 * 128:(t2 + 1) * 128, :], writes=[mt_in])
    for t2 in range(2):
        for half in range(2):
            ps = k.psum()
            for j in range(4):
                kc = half * 4 + j
                k.mm(ps[:, j * 128:(j + 1) * 128], lhsT=mt_in[:, t2 * 1024 + kc * 128:t2 * 1024 + (kc + 1) * 128],
                     rhs=g.c.ident_f[:, :], reads=[mt_in, g.c.ident_f], writes=[ps])
            k.copy("act", memT3[:, half * 4:(half + 1) * 4, t2 * 128:(t2 + 1) * 128], v3(ps[:, :], 4), reads=[ps], writes=[memT])
    sqm = k.alloc(8 * 256, BF16)
    k.act(sqm[:, :], memT[:, :], AF.Square, reads=[memT], writes=[sqm])
    ps = k.psum()
    sqm3 = v3(sqm[:, :], 8)
    for kc in range(8):
        k.mm(ps[:, 0:256], lhsT=g.c.ones_b[:, :], rhs=sqm3[:, kc, :], start=(kc == 0), stop=(kc == 7), reads=[sqm, g.c.ones_b], writes=[ps])
    rm = k.alloc(256)
    k.act(rm[:, :], ps[:, 0:256], AF.Ln, scale=1.0 / D, bias=EPS, reads=[ps], writes=[rm])
    k.act(rm[:, :], rm[:, :], AF.Exp, scale=-0.5, reads=[rm], writes=[rm])
    k.tt("dve", hm3, memT3, rm[:, :].unsqueeze(1).to_broadcast([128, 8, 256]), ALU.mult, reads=[memT, rm], writes=[hm])
    KT3, V3 = v3(KT[:, :], 4), v3(V[:, :], 2)
    kf = k.alloc(256)
    ksq = k.alloc(256, BF16)
    kr = k.alloc(256)
    for hd in range(4):
        ps = k.psum()
        for kc in range(8):
            k.mm(ps[:, 0:256], lhsT=wkv3[:, kc, hd * 128:(hd + 1) * 128], rhs=hm3[:, kc, :], start=(kc == 0), stop=(kc == 7),
                 reads=[wkv, hm], writes=[ps])
        k.copy("act", kf[:, :], ps[:, 0:256], reads=[ps], writes=[kf])
        rs_norm_bcast(k, g, kf, 256, 128, 128, ksq, kr)
        k.stt("dve", KT3[:, hd, :], kf[:, :], kg[:, 0:1], kr[:, :], ALU.mult, ALU.mult, reads=[kf, kg, kr], writes=[KT])
    for t2 in range(2):
        ps = k.psum()
        for kc in range(8):
            k.mm(ps[:, :], lhsT=hm3[:, kc, t2 * 128:(t2 + 1) * 128], rhs=wkv3[:, kc, 512:1024], start=(kc == 0), stop=(kc == 7),
                 reads=[wkv, hm], writes=[ps])
        k.copy("act", V3[:, t2, :], ps[:, :], reads=[ps], writes=[V])
    k.release(m2)
    nx = NormCtx(k, g)
    hT = [k.alloc(8 * TG, BF16) for _ in range(2)]
    qf = k.alloc(TG)
    qsq = k.alloc(TG, BF16)
    qr = k.alloc(TG)
    qn = k.alloc(4 * TG, BF16)
    pT = k.alloc(2 * TG, BF16)
    rec = k.alloc(TG)
    O = k.alloc(4 * TG, BF16)
    qn3, pT3, O3 = v3(qn[:, :], 4), v3(pT[:, :], 2), v3(O[:, :], 4)
    xTv = g.xT.rearrange("(kc p) t -> p kc t", p=128)
    for tg in range(NTG):
        h = hT[tg % 2]
        h3 = v3(h[:, :], 8)
        x3, xtok = nx.run(tg, h3, h)
        for hd in range(4):
            ps = k.psum()
            for kc in range(8):
                k.mm(ps[:, :], lhsT=wq3[:, kc, hd * 128:(hd + 1) * 128], rhs=h3[:, kc, :], start=(kc == 0), stop=(kc == 7),
                     reads=[wq, h], writes=[ps])
            k.copy("act", qf[:, :], ps[:, :], reads=[ps], writes=[qf])
            rs_norm_bcast(k, g, qf, TG, 128, 128, qsq, qr)
            k.stt("dve", qn3[:, hd, :], qf[:, :], qg[:, 0:1], qr[:, :], ALU.mult, ALU.mult, reads=[qf, qg, qr], writes=[qn])
        for hd in range(4):
            for t2 in range(2):
                ps = k.psum()
                k.mm(ps[:, :], lhsT=KT3[:, hd, t2 * 128:(t2 + 1) * 128], rhs=qn3[:, hd, :], reads=[KT, qn], writes=[ps])
                k.act(pT3[:, t2, :], ps[:, :], AF.Exp, reads=[ps], writes=[pT])
            po = k.psum()
            for t2 in range(2):
                k.mm(po[:, :], lhsT=V3[:, t2, hd * 128:(hd + 1) * 128], rhs=pT3[:, t2, :], start=(t2 == 0), stop=(t2 == 1),
                     reads=[V, pT], writes=[po])
            pd = k.psum()
            for t2 in range(2):
                k.mm(pd[:, :], lhsT=g.c.ones_b[:, :], rhs=pT3[:, t2, :], start=(t2 == 0), stop=(t2 == 1),
                     reads=[g.c.ones_b, pT], writes=[pd])
            k.op("dve", lambda e, o=rec[:, :], i_=pd[:, :]: e.reciprocal(out=o, in_=i_), reads=[pd], writes=[rec])
            k.tt("dve", O3[:, hd, :], po[:, :], rec[:, :], ALU.mult, reads=[po, rec], writes=[O])
        for oc in range(8):
            ps = k.psum()
            for hd in range(4):
                k.mm(ps[:, :], lhsT=wo3[:, hd, oc * 128:(oc + 1) * 128], rhs=O3[:, hd, :], start=(hd == 0), stop=(hd == 3),
                     reads=[wo, O], writes=[ps])
            k.tt("dve", x3[:, oc, :], x3[:, oc, :], ps[:, :], ALU.add, reads=[ps, xtok], writes=[xtok])
        k.dma("pool", xTv[:, :, tg * TG:(tg + 1) * TG], x3, reads=[xtok], writes=xtoks(g, tgs=[tg]))
    k.release(m)


def setup_esel(k, g):
    c = g.c
    c.esel = k.alloc(1024)
    k.memset("pool", c.esel[:, :], 1.0, writes=[c.esel])
    k.aselect(c.esel[:, :], c.esel[:, :], [[1, 1024]], ALU.is_ge, 0.0, 0, -128, reads=[c.esel], writes=[c.esel])
    k.aselect(c.esel[:, :], c.esel[:, :], [[-1, 1024]], ALU.is_ge, 0.0, 127, 128, reads=[c.esel], writes=[c.esel])


def stage_ffn(k, g, l):
    moe = (l % 2 == 1)
    j = l // 2
    HC = 11
    TP = 1024
    if moe:
        experts = [(g.moe_wg[j, e], g.moe_wu[j, e], g.moe_wd[j, e]) for e in range(8)]
    else:
        experts = [(g.ffn_wg[j][:, e * 1408:(e + 1) * 1408], g.ffn_wu[j][:, e * 1408:(e + 1) * 1408],
                    g.ffn_wd[j][e * 1408:(e + 1) * 1408, :]) for e in range(2)]
    gn = gain_ap(g, 3, l)
    xTv = g.xT.rearrange("(kc p) t -> p kc t", p=128)
    for half in range(2):
        m = k.mark()
        xres = k.alloc(8 * TP)
        hT = k.alloc(8 * TP, BF16)
        hid = k.alloc(HC * TP, BF16)
        x3, h3, hid3 = v3(xres[:, :], 8), v3(hT[:, :], 8), v3(hid[:, :], HC)
        nx = NormCtx(k, g, own_x=False)
        if moe:
            h32 = k.alloc(8 * TG)
            h32_3 = v3(h32[:, :], 8)
            rst = k.alloc(64)
            rw = k.alloc(64)
            rbias = k.alloc(8)
            gT = k.alloc(TP)
            gbc = k.alloc(TP)
            L = k.alloc(32)
            L2 = k.alloc(32)
            EQ = k.alloc(32)
            m1 = k.alloc(4)
            m2_ = k.alloc(4)
            den = k.alloc(4)
            gts = k.alloc(32)
            k.dma("sp", v3(rst[:, :], 8), g.moe_router[j].rearrange("(kc p) e -> p kc e", p=128), writes=[rst])
            k.tt("dve", v3(rw[:, :], 8), v3(rst[:, :], 8), gn.unsqueeze(2).to_broadcast([128, 8, 8]), ALU.mult,
                 reads=[rst, g.c.gains], writes=[rw])
            k.dma("sp", rbias[:, :], g.moe_bias[j].partition_broadcast(128), writes=[rbias])
            rw3 = v3(rw[:, :], 8)
        for i in range(2):
            tg = half * 2 + i
            sl = slice(i * TG, (i + 1) * TG)
            nx.run(tg, h3[:, :, sl], hT, x3=x3[:, :, sl], xtok=xres, h32_out=h32 if moe else None)
            if moe:
                ps = k.psum()
                for tt in range(4):
                    for kc in range(8):
                        k.mm(ps[:, tt * 8:(tt + 1) * 8], lhsT=h32_3[:, kc, tt * 128:(tt + 1) * 128], rhs=rw3[:, kc, :],
                             start=(kc == 0), stop=(kc == 7), reads=[h32, rw], writes=[ps])
                L3, L23, EQ3, g3 = (v3(b[:, :], 4) for b in (L, L2, EQ, gts))
                k.tt("dve", L3, v3(ps[:, 0:32], 4), rbias[:, :].unsqueeze(1).to_broadcast([128, 4, 8]), ALU.add,
                     reads=[ps, rbias], writes=[L])
                k.op("dve", lambda e, o=m1[:, :], i_=L3: e.tensor_reduce(out=o, in_=i_, axis=AX.X, op=ALU.max), reads=[L], writes=[m1])
                k.tt("dve", EQ3, L3, m1[:, :].unsqueeze(2).to_broadcast([128, 4, 8]), ALU.is_equal, reads=[L, m1], writes=[EQ])
                k.stt("dve", L2[:, :], EQ[:, :], -1e30, L[:, :], ALU.mult, ALU.add, reads=[EQ, L], writes=[L2])
                k.op("dve", lambda e, o=m2_[:, :], i_=L23: e.tensor_reduce(out=o, in_=i_, axis=AX.X, op=ALU.max), reads=[L2], writes=[m2_])
                k.tt("dve", EQ3, L3, m2_[:, :].unsqueeze(2).to_broadcast([128, 4, 8]), ALU.is_ge, reads=[L, m2_], writes=[EQ])
                k.tt("dve", L23, L3, m1[:, :].unsqueeze(2).to_broadcast([128, 4, 8]), ALU.subtract, reads=[L, m1], writes=[L2])
                k.act(L2[:, :], L2[:, :], AF.Exp, reads=[L2], writes=[L2])
                k.tt("dve", den[:, :], m2_[:, :], m1[:, :], ALU.subtract, reads=[m1, m2_], writes=[den])
                k.act(den[:, :], den[:, :], AF.Exp, reads=[den], writes=[den])
                k.ts("dve", den[:, :], den[:, :], 1.0, None, ALU.add, reads=[den], writes=[den])
                k.op("dve", lambda e, o=den[:, :], i_=den[:, :]: e.reciprocal(out=o, in_=i_), reads=[den], writes=[den])
                k.tt("dve", L2[:, :], L2[:, :], EQ[:, :], ALU.mult, reads=[L2, EQ], writes=[L2])
                k.tt("dve", g3, L23, den[:, :].unsqueeze(2).to_broadcast([128, 4, 8]), ALU.mult, reads=[L2, den], writes=[gts])
                pg = k.psum()
                for tt in range(4):
                    k.mm(pg[0:8, tt * 128:(tt + 1) * 128], lhsT=g3[:, tt, :], rhs=g.c.ident_f[:, :], reads=[gts, g.c.ident_f], writes=[pg])
                k.copy("act", gT[0:8, sl], pg[0:8, :], reads=[pg], writes=[gT])
        wst = [k.alloc(8 * 128) for _ in range(4)]
        wbf = [k.alloc(8 * 128, BF16) for _ in range(4)]
        dst = [k.alloc(HC * 128) for _ in range(2)]
        dbf = [k.alloc(HC * 128, BF16) for _ in range(2)]
        sg = [k.alloc(TG) for _ in range(2)]
        wi = 0
        di = 0
        for e, (wg_, wu_, wd_) in enumerate(experts):
            if moe:
                for i in range(2):
                    pb = k.psum()
                    k.mm(pb[:, :], lhsT=g.c.esel[0:8, e * 128:(e + 1) * 128], rhs=gT[0:8, i * TG:(i + 1) * TG],
                         reads=[g.c.esel, gT], writes=[pb])
                    k.copy("act", gbc[:, i * TG:(i + 1) * TG], pb[:, :], reads=[pb], writes=[gbc])
            for hc in range(HC):
                wbs = []
                for wsrc in (wg_, wu_):
                    ws, wb = wst[wi % 4], wbf[wi % 4]
                    k.dma("sp", v3(ws[:, :], 8), wsrc.rearrange("(kc p) c -> p kc c", p=128)[:, :, hc * 128:(hc + 1) * 128], writes=[ws])
                    k.tt("pool", v3(wb[:, :], 8), v3(ws[:, :], 8), gn.unsqueeze(2).to_broadcast([128, 8, 128]), ALU.mult,
                         reads=[ws, g.c.gains], writes=[wb])
                    wbs.append(wb)
                    wi += 1
                for i in range(2):
                    sl = slice(i * TG, (i + 1) * TG)
                    pg_, pu_ = k.psum(), k.psum()
                    for pp, wb in ((pg_, wbs[0]), (pu_, wbs[1])):
                        wb3 = v3(wb[:, :], 8)
                        for kc in range(8):
                            k.mm(pp[:, :], lhsT=wb3[:, kc, :], rhs=h3[:, kc, sl], start=(kc == 0), stop=(kc == 7), reads=[wb, hT], writes=[pp])
                    sb = sg[i]
                    k.act(sb[:, :], pg_[:, :], AF.Silu, reads=[pg_], writes=[sb])
                    if moe:
                        k.tt("pool", sb[:, :], sb[:, :], gbc[:, sl], ALU.mult, reads=[sb, gbc], writes=[sb])
                    k.tt("dve", hid3[:, hc, sl], sb[:, :], pu_[:, :], ALU.mult, reads=[sb, pu_], writes=[hid])
            for oc in range(8):
                ds, db = dst[di % 2], dbf[di % 2]
                di += 1
                k.dma("sp", v3(ds[:, :], HC), wd_.rearrange("(hc p) c -> p hc c", p=128)[:, :, oc * 128:(oc + 1) * 128], writes=[ds])
                k.copy("pool", db[:, :], ds[:, :], reads=[ds], writes=[db])
                db3 = v3(db[:, :], HC)
                for i in range(2):
                    sl = slice(i * TG, (i + 1) * TG)
                    ps = k.psum()
                    for hc in range(HC):
                        k.mm(ps[:, :], lhsT=db3[:, hc, :], rhs=hid3[:, hc, sl], start=(hc == 0), stop=(hc == HC - 1), reads=[db, hid], writes=[ps])
                    k.tt("dve", x3[:, oc, sl], x3[:, oc, sl], ps[:, :], ALU.add, reads=[ps, xres], writes=[xres])
        for i in range(2):
            tg = half * 2 + i
            k.dma("pool", xTv[:, :, tg * TG:(tg + 1) * TG], x3[:, :, i * TG:(i + 1) * TG], reads=[xres], writes=xtoks(g, tgs=[tg]))
        k.release(m)


def setup_rwkv_consts(k, g):
    c = g.c
    c.mUs = k.alloc(512, BF16)
    c.mUi = k.alloc(512, BF16)
    c.mLs = k.alloc(512, BF16)
    c.I8 = k.alloc(512, BF16)
    c.smask = k.alloc(2048, BF16)
    tmp = k.alloc(512)
    t3 = v3(tmp[:, :], 8)
    for dst, cmp, base, cm, pat in ((c.mUs, ALU.is_gt, 0, -1, 1), (c.mUi, ALU.is_ge, 0, -1, 1),
                                    (c.mLs, ALU.is_gt, 0, 1, -1), (c.I8, ALU.is_equal, 0, 1, -1)):
        k.memset("pool", tmp[:, :], 1.0, writes=[tmp])
        k.aselect(t3, t3, [[0, 8], [pat, 64]], cmp, 0.0, base, cm, reads=[tmp], writes=[tmp])
        k.copy("dve", dst[:, :], tmp[:, :], reads=[tmp], writes=[dst])
    k.memset("pool", c.smask[:, :], 1.0, writes=[c.smask])
    k.memset("pool", c.smask[:, :].rearrange("p (a b) -> p a b", b=64)[:, :, 0:1], 0.0, writes=[c.smask])


def stage_rwkv(k, g, l):
    m = k.mark()
    c = g.c
    GS = 256
    NG = T // GS
    NCH = GS // 64
    W = 8 * GS
    pr = k.alloc(16 * 8)
    names = ["mu_r", "mu_k", "mu_v", "w0", "a0", "v0", "kkp", "ka", "rk", "lng", "lnb", "oka"]
    P = {n: pr[0:64, i * 8:(i + 1) * 8] for i, n in enumerate(names)}
    hd = lambda ap: ap.rearrange("(h d) -> d h", d=64)
    mu = g.rwkv_mu[l]
    loads = [("mu_r", hd(mu[0:512])), ("mu_k", hd(mu[512:1024])), ("mu_v", hd(mu[1024:1536])), ("w0", hd(g.rwkv_w0[l])),
             ("a0", hd(g.rwkv_a0[l])), ("kkp", hd(g.rwkv_kk[l])), ("ka", hd(g.rwkv_ka[l])),
             ("rk", g.rwkv_rk[l].rearrange("h d -> d h")), ("lng", hd(g.rwkv_lnx_g[l])), ("lnb", hd(g.rwkv_lnx_b[l]))]
    if l > 0:
        loads.append(("v0", hd(g.rwkv_v0[l - 1])))
    for n, src in loads:
        k.dma("sp", P[n], src, writes=[pr], slow=True)
    mus = k.alloc(4)
    k.dma("sp", mus[0:64, 0:1], mu[1536:1600].rearrange("(p o) -> p o", o=1), writes=[mus])
    k.dma("sp", mus[0:64, 1:2], mu[1600:1664].rearrange("(p o) -> p o", o=1), writes=[mus])
    k.dma("sp", mus[:, 2:3], mu[1664:1792].rearrange("(p o) -> p o", o=1), writes=[mus])
    k.ts("pool", P["oka"], P["ka"], -1.0, 1.0, ALU.mult, ALU.add, reads=[pr], writes=[pr])
    w2b, a2b, g2b, v2b = k.alloc(512, BF16), k.alloc(512, BF16), k.alloc(512, BF16), k.alloc(512, BF16)
    v1b = k.alloc(256, BF16)
    stg = k.alloc(512)
    for dst, src, np_ in ((w2b, g.rwkv_w2[l], 64), (a2b, g.rwkv_a2[l], 64), (g2b, g.rwkv_g2[l], 128)) + \
            (((v2b, g.rwkv_v2[l - 1], 32),) if l > 0 else ()):
        k.dma("sp", stg[0:np_, :], src, writes=[stg])
        k.copy("dve", dst[0:np_, :], stg[0:np_, :], reads=[stg], writes=[dst])
    if l > 0:
        k.dma("sp", v3(stg[0:64, 0:256], 8), g.rwkv_v1[l - 1].rearrange("(h d) r -> d h r", d=64), writes=[stg])
        k.copy("dve", v1b[0:64, :], stg[0:64, 0:256], reads=[stg], writes=[v1b])
    v1b3 = v3(v1b[:, :], 8)
    F = [k.alloc(W) for _ in range(11)]
    Rb, Kb, Vb, AS, KK, K2, BV, BON, TA, TB, YG = F
    curb, prvb = k.alloc(W, BF16), k.alloc(W, BF16)
    AT, RT, BT, KTt, BH, KH, VB, VF = (k.alloc(W, BF16) for _ in range(8))
    wdc, wdp, adc, adp = (k.alloc(GS, BF16) for _ in range(4))
    gdc, gdp = k.alloc(GS, BF16), k.alloc(GS, BF16)
    sm1, sm2, sm3 = k.alloc(GS), k.alloc(GS), k.alloc(GS)
    tw, adb, sgd = k.alloc(GS, BF16), k.alloc(GS, BF16), k.alloc(GS, BF16)
    t32 = k.alloc(GS, BF16)
    PC = k.alloc(8 * NCH)
    Nb = [k.alloc(512, BF16) for _ in range(2)]
    NTb = [k.alloc(512, BF16) for _ in range(2)]
    Tb = [k.alloc(512, BF16) for _ in range(2)]
    Aak, Abr, Akr, VTt, BHT, KHT, W1T, UT, SB = (k.alloc(512, BF16) for _ in range(9))
    SF = k.alloc(512)
    YO = k.alloc(W, BF16)
    k.memset("pool", SF[:, :], 0.0, writes=[SF])
    k.memset("pool", SB[:, :], 0.0, writes=[SB])
    h3 = lambda b: v3(b[0:64, :], 8)
    pj = g.projT

    def load_pair(cur, prv, r0, nrows, gi, heads):
        t0 = gi * GS
        if heads:
            src = pj[r0:r0 + 512, :].rearrange("(h d) t -> d h t", d=64)
            co, po = h3(cur), h3(prv)
        else:
            src = pj[r0:r0 + nrows, :]
            co, po = cur[0:nrows, :], prv[0:nrows, :]
        toks = ptoks(g, r0, r0 + (512 if heads else nrows))
        if heads:
            k.dma("sp", co, src[:, :, t0:t0 + GS], reads=toks, writes=[cur])
            if gi == 0:
                k.memset("pool", po[:, :, 0:1], 0.0, writes=[prv])
                k.dma("sp", po[:, :, 1:GS], src[:, :, 0:GS - 1], reads=toks, writes=[prv])
            else:
                k.dma("sp", po, src[:, :, t0 - 1:t0 + GS - 1], reads=toks, writes=[prv])
        else:
            k.dma("sp", co, src[:, t0:t0 + GS], reads=toks, writes=[cur])
            if gi == 0:
                k.memset("pool", po[:, 0:1], 0.0, writes=[prv])
                k.dma("sp", po[:, 1:GS], src[:, 0:GS - 1], reads=toks, writes=[prv])
            else:
                k.dma("sp", po, src[:, t0 - 1:t0 + GS - 1], reads=toks, writes=[prv])

    def bc(p):
        return p.unsqueeze(2).to_broadcast([64, 8, GS])

    for gi in range(NG):
        t0 = gi * GS
        for dst, r0, mun, eng in ((Rb, 0, "mu_r", "dve"), (Kb, 512, "mu_k", "pool"), (Vb, 1024, "mu_v", "dve")):
            load_pair(curb, prvb, r0, 512, gi, True)
            k.tt(eng, TA[0:64, :], prvb[0:64, :], curb[0:64, :], ALU.subtract, reads=[prvb, curb], writes=[TA])
            k.tt(eng, h3(TA), h3(TA), bc(P[mun]), ALU.mult, reads=[TA, pr], writes=[TA])
            k.tt(eng, dst[0:64, :], TA[0:64, :], curb[0:64, :], ALU.add, reads=[TA, curb], writes=[dst])
        for cur, prv, r0, nr, col, sm in ((wdc, wdp, 1536, 64, 0, sm1), (adc, adp, 1600, 64, 1, sm2), (gdc, gdp, 1664, 128, 2, sm3)):
            load_pair(cur, prv, r0, nr, gi, False)
            k.tt("pool", sm[0:nr, :], prv[0:nr, :], cur[0:nr, :], ALU.subtract, reads=[prv, cur], writes=[sm])
            k.stt("dve", sm[0:nr, :], sm[0:nr, :], mus[0:nr, col:col + 1], cur[0:nr, :], ALU.mult, ALU.add, reads=[sm, mus, cur], writes=[sm])
        k.act(tw[0:64, :], sm1[0:64, :], AF.Tanh, reads=[sm1], writes=[tw])
        k.copy("pool", adb[0:64, :], sm2[0:64, :], reads=[sm2], writes=[adb])
        k.act(sgd[:, :], sm3[:, :], AF.Sigmoid, reads=[sm3], writes=[sgd])
        for dst, wb, rhs, bn, np_ in ((AS, a2b, adb, "a0", 64), (TB, w2b, tw, "w0", 64)):
            for hp in range(4):
                ps = k.psum()
                for q in range(2):
                    h = hp * 2 + q
                    k.mm(ps[0:64, q * GS:(q + 1) * GS], lhsT=wb[0:np_, h * 64:(h + 1) * 64], rhs=rhs[0:np_, :], reads=[wb, rhs], writes=[ps])
                for q in range(2):
                    h = hp * 2 + q
                    k.act(h3(dst)[:, h, :], ps[0:64, q * GS:(q + 1) * GS], AF.Sigmoid, bias=P[bn][:, h:h + 1], reads=[ps, pr], writes=[dst])
        if l > 0:
            k.copy("pool", VB[0:64, :], Vb[0:64, :], reads=[Vb], writes=[VB])
            ps = k.psum()
            for h in range(8):
                k.mm(ps[0:32, 0:GS], lhsT=v1b3[0:64, h, :], rhs=h3(VB)[:, h, :], start=(h == 0), stop=(h == 7), reads=[v1b, VB], writes=[ps])
            k.copy("act", t32[0:32, :], ps[0:32, 0:GS], reads=[ps], writes=[t32])
            for hp in range(4):
                ps = k.psum()
                for q in range(2):
                    h = hp * 2 + q
                    k.mm(ps[0:64, q * GS:(q + 1) * GS], lhsT=v2b[0:32, h * 64:(h + 1) * 64], rhs=t32[0:32, :], reads=[v2b, t32], writes=[ps])
                for q in range(2):
                    h = hp * 2 + q
                    k.act(h3(TA)[:, h, :], ps[0:64, q * GS:(q + 1) * GS], AF.Sigmoid, bias=P["v0"][:, h:h + 1], reads=[ps, pr], writes=[TA])
            k.dma("sp", h3(VF), g.vfT.rearrange("(h d) t -> d h t", d=64)[:, :, t0:t0 + GS], reads=[g.vftok], writes=[VF])
            k.tt("dve", KK[0:64, :], VF[0:64, :], Vb[0:64, :], ALU.subtract, reads=[VF, Vb], writes=[KK])
            k.tt("dve", KK[0:64, :], KK[0:64, :], TA[0:64, :], ALU.mult, reads=[KK, TA], writes=[KK])
            k.tt("dve", Vb[0:64, :], Vb[0:64, :], KK[0:64, :], ALU.add, reads=[Vb, KK], writes=[Vb])
        k.copy("pool", VB[0:64, :], Vb[0:64, :], reads=[Vb], writes=[VB])
        if l == 0:
            k.dma("pool", g.vfT.rearrange("(h d) t -> d h t", d=64)[:, :, t0:t0 + GS], h3(VB), reads=[VB], writes=[g.vftok])
        k.tt("dve", h3(KK), h3(Kb), bc(P["kkp"]), ALU.mult, reads=[Kb, pr], writes=[KK])
        k.act(curb[0:64, :], KK[0:64, :], AF.Square, reads=[KK], writes=[curb])
        for q in range(4):
            ps = k.psum()
            k.mm(ps[0:64, :], lhsT=c.ones_b[0:64, 0:64], rhs=curb[0:64, q * 512:(q + 1) * 512], reads=[c.ones_b, curb], writes=[ps])
            k.act(TA[0:64, q * 512:(q + 1) * 512], ps[0:64, :], AF.Ln, bias=1e-24, reads=[ps], writes=[TA])
        k.act(TA[0:64, :], TA[0:64, :], AF.Exp, scale=-0.5, reads=[TA], writes=[TA])
        k.tt("dve", KK[0:64, :], KK[0:64, :], TA[0:64, :], ALU.mult, reads=[KK, TA], writes=[KK])
        k.tt("pool", h3(K2), h3(AS), bc(P["ka"]), ALU.mult, reads=[AS, pr], writes=[K2])
        k.tt("pool", h3(K2), h3(K2), bc(P["oka"]), ALU.add, reads=[K2, pr], writes=[K2])
        k.tt("pool", K2[0:64, :], K2[0:64, :], Kb[0:64, :], ALU.mult, reads=[K2, Kb], writes=[K2])
        k.tt("dve", BV[0:64, :], KK[0:64, :], AS[0:64, :], ALU.mult, reads=[KK, AS], writes=[BV])
        k.tt("pool", TA[0:64, :], Rb[0:64, :], K2[0:64, :], ALU.mult, reads=[Rb, K2], writes=[TA])
        k.tt("pool", h3(prvb), h3(TA), bc(P["rk"]), ALU.mult, reads=[TA, pr], writes=[prvb])
        for q in range(4):
            ps = k.psum()
            k.mm(ps[0:64, :], lhsT=c.ones_b[0:64, 0:64], rhs=prvb[0:64, q * 512:(q + 1) * 512], reads=[c.ones_b, prvb], writes=[ps])
            k.tt("dve", BON[0:64, q * 512:(q + 1) * 512], ps[0:64, :], Vb[0:64, q * 512:(q + 1) * 512], ALU.mult, reads=[ps, Vb], writes=[BON])
        LW, CL, CLE = TB, AS, Kb
        k.ts("pool", LW[0:64, :], LW[0:64, :], -0.6065306597126334, None, ALU.mult, reads=[LW], writes=[LW])
        k.op("dve", lambda e, o=CL[0:64, :], d0=c.smask[0:64, :], d1=LW[0:64, :]: e.tensor_tensor_scan(
            out=o, data0=d0, data1=d1, initial=0.0, op0=ALU.mult, op1=ALU.add), reads=[c.smask, LW], writes=[CL])
        k.tt("pool", CLE[0:64, :], CL[0:64, :], LW[0:64, :], ALU.subtract, reads=[CL, LW], writes=[CLE])
        CL4 = CL[0:64, :].rearrange("p (h c t) -> p h c t", h=8, c=NCH)
        tot = CL4[:, :, :, 63:64]
        k.act(v3(PC[0:64, :], 8).unsqueeze(3), tot, AF.Exp, reads=[CL], writes=[PC])
        k.tt("pool", TA[0:64, :].rearrange("p (h c t) -> p h c t", h=8, c=NCH), tot.to_broadcast([64, 8, NCH, 64]), CL4, ALU.subtract,
             reads=[CL], writes=[TA])
        k.act(TA[0:64, :], TA[0:64, :], AF.Exp, reads=[TA], writes=[TA])
        k.tt("dve", BH[0:64, :], BV[0:64, :], TA[0:64, :], ALU.mult, reads=[BV, TA], writes=[BH])
        k.tt("pool", KH[0:64, :], K2[0:64, :], TA[0:64, :], ALU.mult, reads=[K2, TA], writes=[KH])
        k.act(TA[0:64, :], CL[0:64, :], AF.Exp, scale=-1.0, reads=[CL], writes=[TA])
        k.tt("dve", BT[0:64, :], BV[0:64, :], TA[0:64, :], ALU.mult, reads=[BV, TA], writes=[BT])
        k.tt("pool", KTt[0:64, :], K2[0:64, :], TA[0:64, :], ALU.mult, reads=[K2, TA], writes=[KTt])
        k.act(TA[0:64, :], CL[0:64, :], AF.Exp, reads=[CL], writes=[TA])
        k.tt("dve", RT[0:64, :], Rb[0:64, :], TA[0:64, :], ALU.mult, reads=[Rb, TA], writes=[RT])
        k.act(TA[0:64, :], CLE[0:64, :], AF.Exp, reads=[CLE], writes=[TA])
        k.stt("dve", AT[0:64, :], KK[0:64, :], -1.0, TA[0:64, :], ALU.mult, ALU.mult, reads=[KK, TA], writes=[AT])
        for ci in range(NCH):
            cs = slice(ci * 64, (ci + 1) * 64)
            A = lambda b, h: h3(b)[:, h, cs]
            blk = lambda b, h: b[0:64, h * 64:(h + 1) * 64]

            def mm8(lhs_b, rhs_b, lf, rf):
                ps = k.psum()
                for h in range(8):
                    k.mm(ps[0:64, h * 64:(h + 1) * 64], lhsT=lf(lhs_b, h), rhs=rf(rhs_b, h), reads=[lhs_b, rhs_b], writes=[ps])
                return ps
            ps = mm8(BT, AT, A, A)
            k.tt("dve", Nb[0][0:64, :], ps[0:64, :], c.mUs[0:64, :], ALU.mult, reads=[ps, c.mUs], writes=[Nb[0]])
            ps = mm8(AT, BT, A, A)
            k.tt("dve", NTb[0][0:64, :], ps[0:64, :], c.mLs[0:64, :], ALU.mult, reads=[ps, c.mLs], writes=[NTb[0]])
            ps = mm8(KTt, AT, A, A)
            k.tt("dve", Aak[0:64, :], ps[0:64, :], c.mUs[0:64, :], ALU.mult, reads=[ps, c.mUs], writes=[Aak])
            ps = mm8(BT, RT, A, A)
            k.tt("dve", Abr[0:64, :], ps[0:64, :], c.mUi[0:64, :], ALU.mult, reads=[ps, c.mUi], writes=[Abr])
            ps = mm8(KTt, RT, A, A)
            k.tt("dve", Akr[0:64, :], ps[0:64, :], c.mUi[0:64, :], ALU.mult, reads=[ps, c.mUi], writes=[Akr])
            for src, dst in ((VB, VTt), (BH, BHT), (KH, KHT)):
                ps = k.psum()
                for h in range(8):
                    k.mm(ps[0:64, h * 64:(h + 1) * 64], lhsT=A(src, h), rhs=c.ident_b[0:64, 0:64], reads=[src, c.ident_b], writes=[ps])
                k.copy("act", dst[0:64, :], ps[0:64, :], reads=[ps], writes=[dst])
            k.tt("pool", Tb[0][0:64, :], Nb[0][0:64, :], c.I8[0:64, :], ALU.add, reads=[Nb[0], c.I8], writes=[Tb[0]])
            cur = 0
            for lvl in range(5):
                nxt = 1 - cur
                if lvl < 4:
                    ps = mm8(NTb[cur], Nb[cur], blk, blk)
                    k.copy("act", Nb[nxt][0:64, :], ps[0:64, :], reads=[ps], writes=[Nb[nxt]])
                ps = mm8(Nb[cur], NTb[cur], blk, blk)
                k.copy("dve", NTb[nxt][0:64, :], ps[0:64, :], reads=[ps], writes=[NTb[nxt]])
                ps = mm8(NTb[nxt], Tb[cur], blk, blk)
                k.tt("dve", Tb[nxt][0:64, :], ps[0:64, :], Tb[cur][0:64, :], ALU.add, reads=[ps, Tb[cur]], writes=[Tb[nxt]])
                cur = nxt
            Tf = Tb[cur]
            ps = k.psum()
            for h in range(8):
                k.mm(ps[0:64, h * 64:(h + 1) * 64], lhsT=A(AT, h), rhs=blk(SB, h), start=True, stop=False, reads=[AT, SB], writes=[ps])
                k.mm(ps[0:64, h * 64:(h + 1) * 64], lhsT=blk(Aak, h), rhs=blk(VTt, h), start=False, stop=True, reads=[Aak, VTt], writes=[ps])
            k.copy("act", W1T[0:64, :], ps[0:64, :], reads=[ps], writes=[W1T])
            ps = mm8(Tf, W1T, blk, blk)
            k.copy("dve", UT[0:64, :], ps[0:64, :], reads=[ps], writes=[UT])
            ps = k.psum()
            for h in range(8):
                o = ps[0:64, h * 64:(h + 1) * 64]
                k.mm(o, lhsT=blk(SB, h), rhs=A(RT, h), start=True, stop=False, reads=[SB, RT], writes=[ps])
                k.mm(o, lhsT=blk(UT, h), rhs=blk(Abr, h), start=False, stop=False, reads=[UT, Abr], writes=[ps])
                k.mm(o, lhsT=blk(VTt, h), rhs=blk(Akr, h), start=False, stop=True, reads=[VTt, Akr], writes=[ps])
            k.copy("act", h3(YG)[:, :, cs], v3(ps[0:64, :], 8), reads=[ps], writes=[YG])
            ps = k.psum()
            for h in range(8):
                o = ps[0:64, h * 64:(h + 1) * 64]
                k.mm(o, lhsT=blk(BHT, h), rhs=blk(UT, h), start=True, stop=False, reads=[BHT, UT], writes=[ps])
                k.mm(o, lhsT=blk(KHT, h), rhs=blk(VTt, h), start=False, stop=True, reads=[KHT, VTt], writes=[ps])
            pcv = v3(PC[0:64, :], 8)[:, :, ci:ci + 1].to_broadcast([64, 8, 64])
            k.tt("dve", v3(SF[0:64, :], 8), v3(SF[0:64, :], 8), pcv, ALU.mult, reads=[SF, PC], writes=[SF])
            k.tt("dve", SF[0:64, :], SF[0:64, :], ps[0:64, :], ALU.add, reads=[SF, ps], writes=[SF])
            k.copy("act", SB[0:64, :], SF[0:64, :], reads=[SF], writes=[SB])
        GG, M, SQ, RS = Rb, Kb, K2, BV
        for hp in range(4):
            ps = k.psum()
            for q in range(2):
                h = hp * 2 + q
                k.mm(ps[0:64, q * GS:(q + 1) * GS], lhsT=g2b[:, h * 64:(h + 1) * 64], rhs=sgd[:, :], reads=[g2b, sgd], writes=[ps])
            k.copy("act", GG[0:64, hp * 512:(hp + 1) * 512], ps[0:64, :], reads=[ps], writes=[GG])
        k.act(SQ[0:64, :], YG[0:64, :], AF.Square, reads=[YG], writes=[SQ])
        for q in range(4):
            qs = slice(q * 512, (q + 1) * 512)
            p1 = k.psum()
            k.mm(p1[0:64, :], lhsT=c.ones_f[0:64, 0:64], rhs=YG[0:64, qs], reads=[c.ones_f, YG], writes=[p1])
            p2 = k.psum()
            k.mm(p2[0:64, :], lhsT=c.ones_f[0:64, 0:64], rhs=SQ[0:64, qs], reads=[c.ones_f, SQ], writes=[p2])
            k.act(M[0:64, qs], p1[0:64, :], AF.Copy, scale=1.0 / 64, reads=[p1], writes=[M])
            k.tt("pool", TA[0:64, qs], M[0:64, qs], M[0:64, qs], ALU.mult, reads=[M], writes=[TA])
            k.stt("dve", RS[0:64, qs], p2[0:64, :], 1.0 / 64, TA[0:64, qs], ALU.mult, ALU.subtract, reads=[p2, TA], writes=[RS])
        k.act(RS[0:64, :], RS[0:64, :], AF.Ln, bias=64e-5, reads=[RS], writes=[RS])
        k.act(RS[0:64, :], RS[0:64, :], AF.Exp, scale=-0.5, reads=[RS], writes=[RS])
        k.tt("dve", TA[0:64, :], YG[0:64, :], M[0:64, :], ALU.subtract, reads=[YG, M], writes=[TA])
        k.tt("dve", TA[0:64, :], TA[0:64, :], RS[0:64, :], ALU.mult, reads=[TA, RS], writes=[TA])
        k.tt("pool", h3(TA), h3(TA), bc(P["lng"]), ALU.mult, reads=[TA, pr], writes=[TA])
        k.tt("pool", h3(TA), h3(TA), bc(P["lnb"]), ALU.add, reads=[TA, pr], writes=[TA])
        k.tt("dve", TA[0:64, :], TA[0:64, :], BON[0:64, :], ALU.add, reads=[TA, BON], writes=[TA])
        k.tt("dve", YO[0:64, :], TA[0:64, :], GG[0:64, :], ALU.mult, reads=[TA, GG], writes=[YO])
        k.dma("pool", g.yaT.rearrange("(h d) t -> d h t", d=64)[:, :, t0:t0 + GS], h3(YO), reads=[YO], writes=[g.yatok])
    k.release(m)


PI_LO = 3.1415925


def setup_dsa_consts(k, g):
    c = g.c
    ra, rb = k.alloc(64), k.alloc(64)
    k.memset("pool", ra[:, :], 1.0, writes=[ra])
    k.aselect(ra[:, :], ra[:, :], [[1, 64]], ALU.is_equal, 0.0, 32, -1, reads=[ra], writes=[ra])
    k.memset("pool", rb[:, :], 1.0, writes=[rb])
    k.aselect(rb[:, :], rb[:, :], [[1, 64]], ALU.is_equal, 0.0, -32, -1, reads=[rb], writes=[rb])
    c.rrot = k.alloc(64, BF16)
    k.tt("dve", c.rrot[:, :], rb[:, :], ra[:, :], ALU.subtract, reads=[ra, rb], writes=[c.rrot])
    c.invf = k.alloc(1)
    k.op("pool", lambda e: e.iota(out=c.invf[:, 0:1], pattern=[[0, 1]], base=0, channel_multiplier=1, allow_small_or_imprecise_dtypes=True), writes=[c.invf])
    pm = k.alloc(1)
    k.ts("dve", pm[:, 0:1], c.invf[:, 0:1], 32.0, -32.0, ALU.is_ge, ALU.mult, reads=[c.invf], writes=[pm])
    k.tt("dve", c.invf[:, 0:1], c.invf[:, 0:1], pm[:, 0:1], ALU.add, reads=[c.invf, pm], writes=[c.invf])
    k.act(c.invf[:, 0:1], c.invf[:, 0:1], AF.Exp, scale=-float(np.log(10000.0)) / 32.0, reads=[c.invf], writes=[c.invf])
    c.hw = k.alloc(24)
    for j in range(24):
        k.memset("pool", c.hw[:, j:j + 1], float(2.0 ** -(j + 1)), writes=[c.hw])


def stage_rope(k, g, s):
    m = k.mark()
    c = g.c
    pi_ = k.alloc(T, I32)
    pf = k.alloc(T)
    a1 = k.alloc(T)
    k.dma("sp", pi_[0:64, :], g.pos[s].partition_broadcast(64), writes=[pi_])
    k.copy("dve", pf[0:64, :], pi_[0:64, :], reads=[pi_], writes=[pf])
    k.ts("dve", pf[0:64, :], pf[0:64, :], c.invf[0:64, 0:1], None, ALU.mult, reads=[pf, c.invf], writes=[pf])
    ni = k.alloc(T, I32)
    nf = k.alloc(T)
    for idx, shift in ((0, float(np.pi / 2)), (1, 0.0)):
        k.ts("dve", a1[0:64, :], pf[0:64, :], shift, None, ALU.add, reads=[pf], writes=[a1])
        k.ts("dve", nf[0:64, :], a1[0:64, :], float(1.0 / (2 * np.pi)), None, ALU.mult, reads=[a1], writes=[nf])
        k.copy("dve", ni[0:64, :], nf[0:64, :], reads=[nf], writes=[ni])
        k.copy("dve", nf[0:64, :], ni[0:64, :], reads=[ni], writes=[nf])
        k.stt("dve", a1[0:64, :], nf[0:64, :], -float(2 * np.pi), a1[0:64, :], ALU.mult, ALU.add, reads=[nf, a1], writes=[a1])
        k.ts("dve", a1[0:64, :], a1[0:64, :], -PI_LO, PI_LO, ALU.max, ALU.min, reads=[a1], writes=[a1])
        k.act(a1[0:64, :], a1[0:64, :], AF.Sin, reads=[a1], writes=[a1])
        k.dma("pool", g.ropeT[idx], a1[0:64, :], reads=[a1], writes=[g.ropetok])
    k.release(m)


def stage_dsa(k, g, l):
    m = k.mark()
    c = g.c
    QB, KB_, VB_, IQB, IKB, IWB = 1792, 2304, 2368, 2432, 2944, 3008
    QT = k.alloc(8 * T, BF16)
    IQ = k.alloc(8 * T, BF16)
    KT = k.alloc(T, BF16)
    IK = k.alloc(T, BF16)
    Vt = k.alloc(16 * 64, BF16)
    IWt = k.alloc(16 * 8)
    QT3, IQ3, Vt3, IWt3 = v3(QT[0:64, :], 8), v3(IQ[0:64, :], 8), v3(Vt[:, :], 16), v3(IWt[:, :], 16)
    gp = k.alloc(4)
    for i, src in enumerate((g.dsa_q_norm[l], g.dsa_k_norm[l], g.idx_k_norm[l])):
        k.dma("sp", gp[0:64, i:i + 1], src.rearrange("(p o) -> p o", o=1), writes=[gp])
    k.ts("pool", gp[0:64, 0:1], gp[0:64, 0:1], 0.125, None, ALU.mult, reads=[gp], writes=[gp])
    m2 = k.mark()
    cs_ = [k.alloc(TG) for _ in range(2)]
    qin = k.alloc(8 * TG, BF16)
    sq = k.alloc(8 * TG, BF16)
    qn = k.alloc(8 * TG, BF16)
    rr = k.alloc(TG)
    t1 = k.alloc(TG)
    t2 = k.alloc(TG)
    smi = k.alloc(TG, BF16)
    qin3, sq3, qn3 = v3(qin[0:64, :], 8), v3(sq[0:64, :], 8), v3(qn[0:64, :], 8)
    pj = g.projT

    def rope_to(dst_ap, src_b, src_ap, dst_tok):
        ps = k.psum()
        k.mm(ps[0:64, :], lhsT=c.rrot[0:64, 0:64], rhs=src_ap, reads=[c.rrot, src_b], writes=[ps])
        k.tt("pool", t1[0:64, :], src_ap, cs_[0][0:64, :], ALU.mult, reads=[src_b, cs_[0]], writes=[t1])
        k.tt("dve", t2[0:64, :], ps[0:64, :], cs_[1][0:64, :], ALU.mult, reads=[ps, cs_[1]], writes=[t2])
        k.tt("pool", dst_ap, t1[0:64, :], t2[0:64, :], ALU.add, reads=[t1, t2], writes=[dst_tok])

    def norm_to(dst_ap, src_ap, src_b, sq_ap, gcol, dst_tok):
        k.act(sq_ap, src_ap, AF.Square, reads=[src_b], writes=[sq])
        ps = k.psum()
        k.mm(ps[0:64, :], lhsT=c.ones_b[0:64, 0:64], rhs=sq_ap, reads=[c.ones_b, sq], writes=[ps])
        k.act(rr[0:64, :], ps[0:64, :], AF.Ln, scale=1.0 / 64, bias=EPS, reads=[ps], writes=[rr])
        k.act(rr[0:64, :], rr[0:64, :], AF.Exp, scale=-0.5, reads=[rr], writes=[rr])
        k.stt("dve", dst_ap, src_ap, gp[0:64, gcol:gcol + 1], rr[0:64, :], ALU.mult, ALU.mult, reads=[src_b, gp, rr], writes=[dst_tok])

    for tg in range(NTG):
        sl = slice(tg * TG, (tg + 1) * TG)
        for i in range(2):
            k.dma("sp", cs_[i][0:64, :], g.ropeT[i][:, sl], reads=[g.ropetok], writes=[cs_[i]])
        k.dma("sp", qin3, pj[QB:QB + 512, :].rearrange("(h d) t -> d h t", d=64)[:, :, sl], reads=ptoks(g, QB, QB + 512), writes=[qin])
        for h in range(8):
            norm_to(qn3[:, h, :], qin3[:, h, :], qin, sq3[:, h, :], 0, qn)
            rope_to(QT3[:, h, sl], qn, qn3[:, h, :], QT)
        k.dma("sp", qin3, pj[IQB:IQB + 512, :].rearrange("(h d) t -> d h t", d=64)[:, :, sl], reads=ptoks(g, IQB, IQB + 512), writes=[qin])
        for h in range(8):
            rope_to(IQ3[:, h, sl], qin, qin3[:, h, :], IQ)
        for r0, gcol, dst in ((KB_, 1, KT), (IKB, 2, IK)):
            k.dma("sp", smi[0:64, :], pj[r0:r0 + 64, sl], reads=ptoks(g, r0, r0 + 64), writes=[smi])
            norm_to(qn3[:, 0, :], smi[0:64, :], smi, sq3[:, 0, :], gcol, qn)
            rope_to(dst[0:64, sl], qn, qn3[:, 0, :], dst)
        k.dma("sp", smi[0:64, :], pj[VB_:VB_ + 64, sl], reads=ptoks(g, VB_, VB_ + 64), writes=[smi])
        ps = k.psum()
        for tt in range(4):
            k.mm(ps[:, tt * 64:(tt + 1) * 64], lhsT=smi[0:64, tt * 128:(tt + 1) * 128], rhs=c.ident_b[0:64, 0:64], reads=[smi, c.ident_b], writes=[ps])
        k.copy("act", Vt3[:, tg * 4:(tg + 1) * 4, :], v3(ps[:, 0:256], 4), reads=[ps], writes=[Vt])
        k.dma("sp", smi[0:8, :], pj[IWB:IWB + 8, sl], reads=ptoks(g, IWB, IWB + 8), writes=[smi])
        ps = k.psum()
        for tt in range(4):
            k.mm(ps[:, tt * 8:(tt + 1) * 8], lhsT=smi[0:8, tt * 128:(tt + 1) * 128], rhs=c.ident_b[0:8, 0:8], reads=[smi, c.ident_b], writes=[ps])
        k.copy("act", IWt3[:, tg * 4:(tg + 1) * 4, :], v3(ps[:, 0:32], 4), reads=[ps], writes=[IWt])
    k.release(m2)
    SC = k.alloc(T)
    junk = k.alloc(T, BF16)
    MASK = k.alloc(T, BF16)
    MT = k.alloc(16 * 128, BF16)
    MT3 = v3(MT[:, :], 16)
    rl = [k.alloc(512) for _ in range(2)]
    PM = [k.alloc(8 * 128, BF16) for _ in range(2)]
    pe_ = [k.alloc(512, BF16) for _ in range(2)]
    lo, hi, wd_, mid, cnt, stp = (k.alloc(1) for _ in range(6))
    hwj = k.alloc(24)
    rec = k.alloc(1024)
    YB = k.alloc(1024, BF16)
    NIT = 17
    for qi in range(16):
        nk = qi + 1
        keys = nk * 128
        qs = slice(qi * 128, (qi + 1) * 128)
        for kg in range((keys + 511) // 512):
            n = min(512, keys - kg * 512)
            ksl = slice(kg * 512, kg * 512 + n)
            for h in range(8):
                ps = k.psum()
                k.mm(ps[:, 0:n], lhsT=IQ3[:, h, qs], rhs=IK[0:64, ksl], reads=[IQ, IK], writes=[ps])
                r_ = rl[h % 2]
                k.act(r_[:, 0:n], ps[:, 0:n], AF.Relu, reads=[ps], writes=[r_])
                if h == 0:
                    k.ts("dve", SC[:, ksl], r_[:, 0:n], IWt3[:, qi, 0:1], None, ALU.mult, reads=[r_, IWt], writes=[SC])
                else:
                    k.stt("dve", SC[:, ksl], r_[:, 0:n], IWt3[:, qi, h:h + 1], SC[:, ksl], ALU.mult, ALU.add, reads=[r_, IWt, SC], writes=[SC])
        if qi >= 2:
            k.op("dve", lambda e, o=lo[:, 0:1], i_=SC[:, 0:keys]: e.tensor_reduce(out=o, in_=i_, axis=AX.X, op=ALU.min), reads=[SC], writes=[lo])
            k.op("dve", lambda e, o=hi[:, 0:1], i_=SC[:, 0:keys]: e.tensor_reduce(out=o, in_=i_, axis=AX.X, op=ALU.max), reads=[SC], writes=[hi])
        k.memset("pool", SC[0:64, qi * 128 + 64:(qi + 1) * 128], -1e30, writes=[SC])
        if qi < 2:
            k.ts("dve", MASK[:, 0:keys], SC[:, 0:keys], -1e29, None, ALU.is_gt, reads=[SC], writes=[MASK])
        else:
            k.tt("dve", wd_[:, 0:1], hi[:, 0:1], lo[:, 0:1], ALU.subtract, reads=[hi, lo], writes=[wd_])
            k.ts("dve", wd_[:, 0:1], wd_[:, 0:1], 1.001, 1e-6, ALU.mult, ALU.add, reads=[wd_], writes=[wd_])
            k.ts("dve", hwj[:, 0:NIT], c.hw[:, 0:NIT], wd_[:, 0:1], None, ALU.mult, reads=[c.hw, wd_], writes=[hwj])
            for j in range(NIT):
                k.tt("dve", mid[:, 0:1], lo[:, 0:1], hwj[:, j:j + 1], ALU.add, reads=[lo, hwj], writes=[mid])
                k.ts("dve", junk[:, 0:keys], SC[:, 0:keys], mid[:, 0:1], None, ALU.is_ge, ALU.add, reads=[SC, mid], writes=[junk, cnt],
                     accum=cnt[:, 0:1])
                k.ts("dve", stp[:, 0:1], cnt[:, 0:1], 255.5, hwj[:, j:j + 1], ALU.is_ge, ALU.mult, reads=[cnt, hwj], writes=[stp])
                k.tt("dve", lo[:, 0:1], lo[:, 0:1], stp[:, 0:1], ALU.add, reads=[lo, stp], writes=[lo])
            k.ts("dve", MASK[:, 0:keys], SC[:, 0:keys], lo[:, 0:1], None, ALU.is_ge, reads=[SC, lo], writes=[MASK])
        for kb in range((nk + 3) // 4):
            ps = k.psum()
            nn = min(4, nk - kb * 4)
            for j in range(nn):
                kt = kb * 4 + j
                k.mm(ps[:, j * 128:(j + 1) * 128], lhsT=MASK[:, kt * 128:(kt + 1) * 128], rhs=c.ident_b[:, :], reads=[MASK, c.ident_b], writes=[ps])
            k.copy("act", MT3[:, kb * 4:kb * 4 + nn, :], v3(ps[:, 0:nn * 128], nn), reads=[ps], writes=[MT])
        k.reserved = {0, 1, 2, 3}
        po = [k.psum_fixed(0), k.psum_fixed(1)]
        pd = [k.psum_fixed(2), k.psum_fixed(3)]
        for kt in range(nk):
            pm = PM[kt % 2]
            pm3 = v3(pm[:, :], 8)
            for hh in range(2):
                ps = k.psum()
                for q4 in range(4):
                    h = hh * 4 + q4
                    k.mm(ps[:, q4 * 128:(q4 + 1) * 128], lhsT=KT[0:64, kt * 128:(kt + 1) * 128], rhs=QT3[:, h, qs], reads=[KT, QT], writes=[ps])
                pe = pe_[hh]
                k.act(pe[:, :], ps[:, :], AF.Exp, reads=[ps], writes=[pe])
                k.tt("dve" if hh == 0 else "pool", pm3[:, hh * 4:(hh + 1) * 4, :], v3(pe[:, :], 4),
                     MT3[:, kt, :].unsqueeze(1).to_broadcast([128, 4, 128]), ALU.mult, reads=[pe, MT], writes=[pm])
            for hh in range(2):
                k.mm(po[hh][0:64, :], lhsT=Vt3[:, kt, :], rhs=pm[:, hh * 512:(hh + 1) * 512], start=(kt == 0), stop=(kt == nk - 1),
                     reads=[Vt, pm], writes=[po[hh]])
                k.mm(pd[hh][0:64, :], lhsT=c.ones_b[:, 0:64], rhs=pm[:, hh * 512:(hh + 1) * 512], start=(kt == 0), stop=(kt == nk - 1),
                     reads=[c.ones_b, pm], writes=[pd[hh]])
        for hh in range(2):
            k.op("dve", lambda e, o=rec[0:64, hh * 512:(hh + 1) * 512], i_=pd[hh][0:64, :]: e.reciprocal(out=o, in_=i_), reads=[pd[hh]], writes=[rec])
            k.tt("dve", YB[0:64, hh * 512:(hh + 1) * 512], po[hh][0:64, :], rec[0:64, hh * 512:(hh + 1) * 512], ALU.mult,
                 reads=[po[hh], rec], writes=[YB])
        k.reserved = set()
        k.dma("pool", g.ybT.rearrange("(h d) t -> d h t", d=64)[:, :, qs], v3(YB[0:64, :], 8), reads=[YB], writes=[g.ybtok])
    k.release(m)


def build(NS, layers=(0, 1, 2, 3), stages="armdxf", dbg=False, ext_in=()):
    nc = bass.Bass("TRN2", target_bir_lowering=False)
    g = setup(nc, NS, dbg, ext_in)
    g.outtok = Tok()
    k = KB(nc)
    setup_consts(k, g)
    setup_esel(k, g)
    setup_rwkv_consts(k, g)
    setup_dsa_consts(k, g)
    for s in range(NS):
        if "xT" not in ext_in:
            stage_x0(k, g, s)
        if "d" in stages and "ropeT" not in ext_in:
            stage_rope(k, g, s)
        for l in layers:
            if "a" in stages:
                stage_a(k, g, l)
            if "r" in stages:
                stage_rwkv(k, g, l)
            if "d" in stages:
                stage_dsa(k, g, l)
            if "m" in stages:
                stage_merge(k, g, l)
            if "x" in stages:
                stage_mem(k, g, l, s)
            if "f" in stages:
                stage_ffn(k, g, l)
        stage_final(k, g, s)
    k.finish()
    return nc, k, g


_CACHE = {}


def kernel(**inputs):
    n_cores = 8
    NS = 4
    if "prog" not in _CACHE:
        _CACHE["prog"] = build(NS)
    nc, kb_, g = _CACHE["prog"]
    x = np.ascontiguousarray(np.asarray(inputs["x"], dtype=np.float32))
    mem = np.ascontiguousarray(np.asarray(inputs["mem"], dtype=np.float32))
    pos = np.ascontiguousarray(np.asarray(inputs["positions"]).astype(np.int32))
    wmap = {name: np.ascontiguousarray(np.asarray(inputs[name], dtype=np.float32)) for name in g.W}
    in_maps = []
    for i in range(n_cores):
        m = {"x": x[i * NS:(i + 1) * NS], "mem": mem[i * NS:(i + 1) * NS], "pos": pos[i * NS:(i + 1) * NS]}
        m.update(wmap)
        in_maps.append(m)
    res = run_bass_kernel_spmd(nc, in_maps, core_ids=list(range(n_cores)))
    return np.concatenate([np.asarray(r["out"], dtype=np.float32) for r in res.results], axis=0)
```
